# Optimizing a Trainium2 kernel written in Bass

```python
import math
import jax, jax.numpy as jnp
from jax import lax
import numpy as np

D_MODEL = 2048
BATCH = 4
SEQ = 2048
DEPTH = 1
DEC_BATCH = 128
DEC_SEQ = 1
PAST_LEN = 16384
PAGE_SIZE = 128

N_META = 16
D_RNN = D_MODEL // 2
RG_HEADS = 8
RG_BLOCK = D_RNN // RG_HEADS
CONV_W = 4
LRU_C = 8.0
S5_CH = 16
S5_GROUPS = (D_MODEL // 2) // S5_CH
D_S5 = S5_GROUPS * S5_CH
S5_N = 64
N_IN = 2 * D_RNN + D_S5 + 2 * D_MODEL
N_EXPERTS = 64
TOP_K = 8
N_EXPERT_GROUPS = 8
TOPK_GROUPS = 4
D_EXPERT = 512
D_SHARED = D_EXPERT
ROUTED_SCALE = 2.5
LN_EPS = 1e-5

kernel_name = "hawk_s5_moe_deepnorm_meta_step"

F32 = jnp.float32


def _layernorm(x, g, b):
    xf = x.astype(F32)
    mu = jnp.mean(xf, axis=-1, keepdims=True)
    xc = xf - mu
    var = jnp.mean(xc * xc, axis=-1, keepdims=True)
    y = xc * lax.rsqrt(var + LN_EPS) * g.astype(F32) + b.astype(F32)
    return y.astype(x.dtype)


def _causal_conv(u, buf, w, b):
    T = u.shape[1]
    full = jnp.concatenate([buf.astype(u.dtype), u], axis=1)
    out = b + full[:, 0:T] * w[0]
    for k in range(1, CONV_W):
        out = out + full[:, k:k + T] * w[k]
    return out, full[:, -(CONV_W - 1):]


def _rglru(xc, h0, wa, ba, wi, bi, lam):
    B, T, _ = xc.shape
    xf = xc.astype(F32)
    xh = xf.reshape(B, T, RG_HEADS, RG_BLOCK)
    r = jax.nn.sigmoid(jnp.einsum('bthi,hij->bthj', xh, wa.astype(F32)).reshape(B, T, D_RNN) + ba.astype(F32))
    i = jax.nn.sigmoid(jnp.einsum('bthi,hij->bthj', xh, wi.astype(F32)).reshape(B, T, D_RNN) + bi.astype(F32))
    log_a = -LRU_C * r * jax.nn.softplus(-lam.astype(F32))
    a = jnp.exp(log_a)
    u = jnp.sqrt(-jnp.expm1(2.0 * log_a)) * (i * xf)

    def step(h, au):
        a_t, u_t = au
        h = a_t * h + u_t
        return h, h

    hT, hs = lax.scan(step, h0.astype(F32), (jnp.swapaxes(a, 0, 1), jnp.swapaxes(u, 0, 1)))
    return jnp.swapaxes(hs, 0, 1), hT


def _cplx_combine(e1, e2):
    a1r, a1i, b1r, b1i = e1
    a2r, a2i, b2r, b2i = e2
    return (a2r * a1r - a2i * a1i,
            a2r * a1i + a2i * a1r,
            a2r * b1r - a2i * b1i + b2r,
            a2r * b1i + a2i * b1r + b2i)


def _s5(u, h0, a_re, a_im, b_re, b_im, c_re, c_im, d, log_dt):
    B, T, _ = u.shape
    uf = u.astype(F32).reshape(B, T, S5_GROUPS, S5_CH)
    a_re = a_re.astype(F32); a_im = a_im.astype(F32)
    b_re = b_re.astype(F32); b_im = b_im.astype(F32)
    dt = jnp.exp(log_dt.astype(F32))[:, None]
    mag = jnp.exp(a_re * dt)
    ab_r = mag * jnp.cos(a_im * dt)
    ab_i = mag * jnp.sin(a_im * dt)
    den = a_re * a_re + a_im * a_im
    nr = ab_r - 1.0
    cr = (nr * a_re + ab_i * a_im) / den
    ci = (ab_i * a_re - nr * a_im) / den
    bb_r = cr[..., None] * b_re - ci[..., None] * b_im
    bb_i = cr[..., None] * b_im + ci[..., None] * b_re
    bu_r = jnp.einsum('btgc,gnc->btgn', uf, bb_r)
    bu_i = jnp.einsum('btgc,gnc->btgn', uf, bb_i)
    ar = jnp.broadcast_to(ab_r, bu_r.shape)
    ai = jnp.broadcast_to(ab_i, bu_i.shape)
    acum_r, acum_i, hr, hi = lax.associative_scan(_cplx_combine, (ar, ai, bu_r, bu_i), axis=1)
    if h0 is not None:
        h0r = h0[0].astype(F32)[:, None]
        h0i = h0[1].astype(F32)[:, None]
        hr = hr + acum_r * h0r - acum_i * h0i
        hi = hi + acum_r * h0i + acum_i * h0r
    y = (jnp.einsum('gcn,btgn->btgc', c_re.astype(F32), hr)
         - jnp.einsum('gcn,btgn->btgc', c_im.astype(F32), hi)
         + d.astype(F32) * uf)
    return y.reshape(B, T, D_S5), hr[:, -1], hi[:, -1]


def _swiglu(x, w1, w3, w2):
    return (jax.nn.silu(x @ w1) * (x @ w3)) @ w2


def _moe(x, p):
    B, T, D = x.shape
    xt = x.reshape(-1, D)
    scores = jax.nn.sigmoid((xt @ p['router_w']).astype(F32))
    sel = scores + p['router_bias'].astype(F32)
    grp = sel.reshape(-1, N_EXPERT_GROUPS, N_EXPERTS // N_EXPERT_GROUPS)
    gscore = jnp.sum(lax.top_k(grp, 2)[0], axis=-1)
    _, gidx = lax.top_k(gscore, TOPK_GROUPS)
    gmask = jnp.sum(jax.nn.one_hot(gidx, N_EXPERT_GROUPS, dtype=F32), axis=-2) > 0
    masked = jnp.where(gmask[..., None], grp, -jnp.inf).reshape(-1, N_EXPERTS)
    _, eidx = lax.top_k(masked, TOP_K)
    w = jnp.take_along_axis(scores, eidx, axis=-1)
    w = w / jnp.sum(w, axis=-1, keepdims=True) * ROUTED_SCALE
    gates = jnp.sum(jax.nn.one_hot(eidx, N_EXPERTS, dtype=F32) * w[..., None], axis=-2).astype(x.dtype)
    out = _swiglu(xt, p['sh_w1'], p['sh_w3'], p['sh_w2'])
    for e in range(N_EXPERTS):
        out = out + gates[:, e:e + 1] * _swiglu(xt, p['ex_w1'][e], p['ex_w3'][e], p['ex_w2'][e])
    return out.reshape(B, T, D)


def _layer(x, rg_h0, conv0, s5_h0, p, alpha):
    dt = x.dtype
    z = x @ p['w_in'] + p['b_in']
    o1, o2, o3, o4 = D_RNN, 2 * D_RNN, 2 * D_RNN + D_S5, 2 * D_RNN + D_S5 + D_MODEL
    xa, ya, us, ga, gb = z[..., :o1], z[..., o1:o2], z[..., o2:o3], z[..., o3:o4], z[..., o4:]
    xc, conv_new = _causal_conv(xa, conv0, p['conv_w'], p['conv_b'])
    hs, rg_hT = _rglru(xc, rg_h0, p['rg_wa'], p['rg_ba'], p['rg_wi'], p['rg_bi'], p['rg_lambda'])
    branch_a = (hs * jax.nn.gelu(ya.astype(F32))).astype(dt) @ p['proj_a']
    y5, s5r, s5i = _s5(us, s5_h0, p['s5_a_re'], p['s5_a_im'], p['s5_b_re'], p['s5_b_im'],
                       p['s5_c_re'], p['s5_c_im'], p['s5_d'], p['s5_log_dt'])
    g5 = jax.nn.gelu(y5)
    glu = g5 * jax.nn.sigmoid(g5 @ p['glu_w'].astype(F32) + p['glu_b'].astype(F32))
    branch_b = glu.astype(dt) @ p['proj_b']
    merged = jax.nn.sigmoid(ga) * branch_a + jax.nn.sigmoid(gb) * branch_b
    x = _layernorm(alpha * x + merged @ p['w_o'], p['ln1_g'], p['ln1_b'])
    x = _layernorm(alpha * x + _moe(x, p), p['ln2_g'], p['ln2_b'])
    return x, (rg_hT.astype(dt), conv_new, s5r.astype(dt), s5i.astype(dt))


def setup_inputs(seed: int = 0) -> dict:
    key = jax.random.key(seed)
    ks = iter(jax.random.split(key, 64))
    nrm = lambda shape, s=1.0: jax.random.normal(next(ks), shape, F32) * s
    beta = (8.0 * DEPTH) ** -0.25
    L = DEPTH
    a0 = jax.random.uniform(next(ks), (L, D_RNN), F32, 0.9, 0.999)
    inp = {}
    inp['x_prompt'] = nrm((BATCH, SEQ, D_MODEL))
    inp['x_sample'] = nrm((DEC_BATCH, DEC_SEQ, D_MODEL))
    inp['state_rglru_h'] = nrm((L, DEC_BATCH, D_RNN), 0.5)
    inp['state_conv'] = nrm((L, DEC_BATCH, CONV_W - 1, D_RNN))
    inp['state_s5_re'] = nrm((L, DEC_BATCH, S5_GROUPS, S5_N), 0.3)
    inp['state_s5_im'] = nrm((L, DEC_BATCH, S5_GROUPS, S5_N), 0.3)
    inp['meta_tokens'] = nrm((N_META, D_MODEL))
    inp['ln_in_g'] = 1.0 + nrm((D_MODEL,), 0.02)
    inp['ln_in_b'] = nrm((D_MODEL,), 0.02)
    inp['w_in'] = nrm((L, D_MODEL, N_IN), D_MODEL ** -0.5)
    inp['b_in'] = nrm((L, N_IN), 0.02)
    inp['conv_w'] = nrm((L, CONV_W, D_RNN), CONV_W ** -0.5)
    inp['conv_b'] = nrm((L, D_RNN), 0.02)
    inp['rg_wa'] = nrm((L, RG_HEADS, RG_BLOCK, RG_BLOCK), RG_BLOCK ** -0.5)
    inp['rg_ba'] = nrm((L, D_RNN), 0.02)
    inp['rg_wi'] = nrm((L, RG_HEADS, RG_BLOCK, RG_BLOCK), RG_BLOCK ** -0.5)
    inp['rg_bi'] = nrm((L, D_RNN), 0.02)
    inp['rg_lambda'] = jnp.log(a0) - jnp.log1p(-a0)
    inp['s5_a_re'] = -0.5 + nrm((L, S5_GROUPS, S5_N), 0.01)
    inp['s5_a_im'] = jnp.pi * jnp.arange(S5_N, dtype=F32) + nrm((L, S5_GROUPS, S5_N), 0.01)
    inp['s5_b_re'] = nrm((L, S5_GROUPS, S5_N, S5_CH), (2 * S5_CH) ** -0.5)
    inp['s5_b_im'] = nrm((L, S5_GROUPS, S5_N, S5_CH), (2 * S5_CH) ** -0.5)
    inp['s5_c_re'] = nrm((L, S5_GROUPS, S5_CH, S5_N), (2 * S5_N) ** -0.5)
    inp['s5_c_im'] = nrm((L, S5_GROUPS, S5_CH, S5_N), (2 * S5_N) ** -0.5)
    inp['s5_d'] = nrm((L, S5_GROUPS, S5_CH))
    inp['s5_log_dt'] = jax.random.uniform(next(ks), (L, S5_GROUPS), F32, math.log(0.001), math.log(0.1))
    inp['glu_w'] = nrm((L, D_S5, D_S5), D_S5 ** -0.5)
    inp['glu_b'] = nrm((L, D_S5), 0.02)
    inp['proj_a'] = nrm((L, D_RNN, D_MODEL), D_RNN ** -0.5)
    inp['proj_b'] = nrm((L, D_S5, D_MODEL), D_S5 ** -0.5)
    inp['w_o'] = nrm((L, D_MODEL, D_MODEL), beta * D_MODEL ** -0.5)
    inp['ln1_g'] = 1.0 + nrm((L, D_MODEL), 0.02)
    inp['ln1_b'] = nrm((L, D_MODEL), 0.02)
    inp['router_w'] = nrm((L, D_MODEL, N_EXPERTS), D_MODEL ** -0.5)
    inp['router_bias'] = nrm((L, N_EXPERTS), 0.01)
    inp['ex_w1'] = nrm((L, N_EXPERTS, D_MODEL, D_EXPERT), D_MODEL ** -0.5)
    inp['ex_w3'] = nrm((L, N_EXPERTS, D_MODEL, D_EXPERT), D_MODEL ** -0.5)
    inp['ex_w2'] = nrm((L, N_EXPERTS, D_EXPERT, D_MODEL), beta * D_EXPERT ** -0.5)
    inp['sh_w1'] = nrm((L, D_MODEL, D_SHARED), D_MODEL ** -0.5)
    inp['sh_w3'] = nrm((L, D_MODEL, D_SHARED), D_MODEL ** -0.5)
    inp['sh_w2'] = nrm((L, D_SHARED, D_MODEL), beta * D_SHARED ** -0.5)
    inp['ln2_g'] = 1.0 + nrm((L, D_MODEL), 0.02)
    inp['ln2_b'] = nrm((L, D_MODEL), 0.02)
    return inp


def reference(x_prompt, x_sample, state_rglru_h, state_conv, state_s5_re, state_s5_im,
              meta_tokens, ln_in_g, ln_in_b, w_in, b_in, conv_w, conv_b,
              rg_wa, rg_ba, rg_wi, rg_bi, rg_lambda,
              s5_a_re, s5_a_im, s5_b_re, s5_b_im, s5_c_re, s5_c_im, s5_d, s5_log_dt,
              glu_w, glu_b, proj_a, proj_b, w_o, ln1_g, ln1_b,
              router_w, router_bias, ex_w1, ex_w3, ex_w2, sh_w1, sh_w3, sh_w2, ln2_g, ln2_b):
    alpha = (2.0 * DEPTH) ** 0.25
    bp = x_prompt.shape[0]
    meta = jnp.broadcast_to(meta_tokens.astype(x_prompt.dtype)[None], (bp, N_META, D_MODEL))
    xp = _layernorm(jnp.concatenate([meta, x_prompt], axis=1), ln_in_g, ln_in_b)
    xs = _layernorm(x_sample, ln_in_g, ln_in_b)
    p_h, p_c, p_r, p_i = [], [], [], []
    s_h, s_c, s_r, s_i = [], [], [], []
    for l in range(DEPTH):
        p = dict(w_in=w_in[l], b_in=b_in[l], conv_w=conv_w[l], conv_b=conv_b[l],
                 rg_wa=rg_wa[l], rg_ba=rg_ba[l], rg_wi=rg_wi[l], rg_bi=rg_bi[l], rg_lambda=rg_lambda[l],
                 s5_a_re=s5_a_re[l], s5_a_im=s5_a_im[l], s5_b_re=s5_b_re[l], s5_b_im=s5_b_im[l],
                 s5_c_re=s5_c_re[l], s5_c_im=s5_c_im[l], s5_d=s5_d[l], s5_log_dt=s5_log_dt[l],
                 glu_w=glu_w[l], glu_b=glu_b[l], proj_a=proj_a[l], proj_b=proj_b[l], w_o=w_o[l],
                 ln1_g=ln1_g[l], ln1_b=ln1_b[l], router_w=router_w[l], router_bias=router_bias[l],
                 ex_w1=ex_w1[l], ex_w3=ex_w3[l], ex_w2=ex_w2[l],
                 sh_w1=sh_w1[l], sh_w3=sh_w3[l], sh_w2=sh_w2[l], ln2_g=ln2_g[l], ln2_b=ln2_b[l])
        h0p = jnp.zeros((bp, D_RNN), xp.dtype)
        c0p = jnp.zeros((bp, CONV_W - 1, D_RNN), xp.dtype)
        xp, (ph, pc, pr, pi) = _layer(xp, h0p, c0p, None, p, alpha)
        xs, (sh, sc, sr, si) = _layer(xs, state_rglru_h[l], state_conv[l],
                                      (state_s5_re[l], state_s5_im[l]), p, alpha)
        p_h.append(ph); p_c.append(pc); p_r.append(pr); p_i.append(pi)
        s_h.append(sh); s_c.append(sc); s_r.append(sr); s_i.append(si)
    y_prompt = xp[:, N_META:]
    y_sample = xs
    return (y_prompt, y_sample,
            jnp.stack(p_h), jnp.stack(p_c), jnp.stack(p_r), jnp.stack(p_i),
            jnp.stack(s_h), jnp.stack(s_c), jnp.stack(s_r), jnp.stack(s_i))
```

```python
import math
from contextlib import ExitStack

import numpy as np
import concourse.bass as bass
import concourse.mybir as mybir
from concourse.bass_utils import run_bass_kernel_spmd

F32 = mybir.dt.float32
BF16 = mybir.dt.bfloat16
AF = mybir.ActivationFunctionType
ALU = mybir.AluOpType

D = 2048
NT = 1048
NPR = 1032
NS = 16
LCH = 258
DR = 1024
NE = 64
ALPHA = 2.0 ** 0.25
EPS = 1e-5
MAGIC = 12582912.0
TWO_PI = 2.0 * math.pi
AW = 51200

ENGS = ("pe", "act", "dve", "pool", "sp")


class Buf:
    __slots__ = ("name", "w", "r", "sem", "semval")

    def __init__(self, name=""):
        self.name = name
        self.w = None
        self.r = []
        self.sem = None
        self.semval = 0


class Prog:
    def __init__(self, nc):
        self.nc = nc
        self.ops = {e: [] for e in ENGS}
        self.esem = {}
        self.nsem = 0
        self.floor = []
        self.dma_toks = []

    def _newsem(self, name):
        self.nsem += 1
        return self.nc.alloc_semaphore(name=f"{name}_{self.nsem}")

    def _deps(self, eng, reads, writes):
        deps = list(self.floor)
        for b in reads:
            if b.w is not None:
                deps.append(b.w)
        for b in writes:
            if b.w is not None:
                deps.append(b.w)
            deps.extend(b.r)
        out = []
        seen = set()
        for t in deps:
            if t in seen:
                continue
            seen.add(t)
            if t[0] == "e" and t[1] == "pe" and eng == "pe":
                continue
            out.append(t)
        for t in out:
            if t[0] == "e":
                self.ops[t[1]][t[2]]["sig"] = True
        return out

    def op(self, eng, fn, reads=(), writes=()):
        deps = self._deps(eng, reads, writes)
        idx = len(self.ops[eng])
        self.ops[eng].append({"fn": fn, "deps": deps, "sig": False, "dma": None})
        tok = ("e", eng, idx)
        for b in reads:
            b.r.append(tok)
        for b in writes:
            b.w = tok
            b.r = []
        return tok

    def dma(self, q, fn, reads=(), writes=(), chan=None):
        deps = self._deps(q, reads, writes)
        if chan is None:
            chan = writes[0]
        if chan.sem is None:
            chan.sem = self._newsem("d")
        chan.semval += 16
        tok = ("d", chan.sem, chan.semval)
        self.ops[q].append({"fn": fn, "deps": deps, "sig": False, "dma": (chan.sem, 16)})
        for b in reads:
            b.r.append(tok)
        for b in writes:
            b.w = tok
            b.r = []
        self.dma_toks.append(tok)
        return tok

    def barrier(self):
        fl = []
        for e in ("pe", "act", "dve", "pool"):
            if self.ops[e]:
                for i in range(len(self.ops[e]) - 1, -1, -1):
                    if self.ops[e][i]["dma"] is None:
                        self.ops[e][i]["sig"] = True
                        fl.append(("e", e, i))
                        break
        fl.extend(self.dma_toks)
        self.dma_toks = []
        self.floor = fl

    def emit(self, final_tokens=()):
        nc = self.nc
        for e in ("pe", "act", "dve", "pool"):
            self.esem[e] = self._newsem("e" + e)
        cnt = {}
        for e in ENGS:
            c = 0
            arr = []
            for o in self.ops[e]:
                if o["sig"]:
                    c += 1
                arr.append(c)
            cnt[e] = arr
        prog = self

        def run(e, eng):
            known = {}
            for o in prog.ops[e]:
                for t in o["deps"]:
                    if t[0] == "e":
                        v = cnt[t[1]][t[2]]
                        key = ("e", t[1])
                        if known.get(key, 0) >= v:
                            continue
                        known[key] = v
                        eng.wait_ge(prog.esem[t[1]], v)
                    else:
                        key = ("d", id(t[1]))
                        if known.get(key, 0) >= t[2]:
                            continue
                        known[key] = t[2]
                        eng.wait_ge(t[1], t[2])
                ins = o["fn"](eng)
                if o["dma"] is not None:
                    ins.then_inc(o["dma"][0], o["dma"][1])
                elif o["sig"]:
                    ins.then_inc(prog.esem[e], 1)
            if e == "sp":
                for t in final_tokens:
                    if t[0] == "d":
                        eng.wait_ge(t[1], t[2])
                    else:
                        eng.wait_ge(prog.esem[t[1]], cnt[t[1]][t[2]])

        with nc.Block() as block:
            @block.sync
            def _(eng):
                run("sp", eng)

            @block.tensor
            def _(eng):
                run("pe", eng)

            @block.scalar
            def _(eng):
                run("act", eng)

            @block.vector
            def _(eng):
                run("dve", eng)

            @block.gpsimd
            def _(eng):
                run("pool", eng)


def _blocks(n):
    out = []
    s = 0
    while s < n:
        w = min(512, n - s)
        out.append((s, w))
        s += w
    return out


def _tiles(n):
    out = []
    s = 0
    while s < n:
        w = min(128, n - s)
        out.append((s, w))
        s += w
    return out


def b3(ap2, n):
    P_, a = ap2.shape
    return ap2.rearrange("p (a o) -> p a o", o=1).to_broadcast([P_, a, n])


class K:
    def __init__(self, stage=99, dbg=()):
        self.stage = stage
        self.dbg_names = dbg
        self.nc = bass.Bass("TRN2", target_bir_lowering=False)
        self.P = Prog(self.nc)
        self.es = ExitStack()
        self.final = []
        self.dbg_specs = {}

    def din(self, name, shape):
        return self.nc.dram_tensor(name, list(shape), F32, kind="ExternalInput").ap()

    def dout(self, name, shape, dt=F32):
        return self.nc.dram_tensor(name, list(shape), dt, kind="ExternalOutput").ap()

    def sb(self, name, shape, dt=F32):
        return self.es.enter_context(self.nc.sbuf_tensor(name, list(shape), dt))

    def I(self, eng, name, reads, writes, *a, **kw):
        return self.P.op(eng, lambda e: getattr(e, name)(*a, **kw), reads, writes)

    def act(self, out, in_, func, reads, writes, bias=None, scale=None):
        kw = {}
        if bias is not None:
            kw["bias"] = bias
        if scale is not None:
            kw["scale"] = scale
        return self.P.op("act", lambda e: e.activation(out, in_, func, **kw), reads, writes)

    def mm(self, out, lhsT, rhs, start, stop, reads, writes):
        return self.P.op("pe", lambda e: e.matmul(out, lhsT, rhs, start=start, stop=stop), reads, writes)

    def tr(self, out, in_, ident, reads, writes):
        return self.P.op("pe", lambda e: e.transpose(out, in_, ident), reads, writes)

    def ld(self, out, in_, writes, reads=(), q="sp"):
        return self.P.dma(q, lambda e: e.dma_start(out=out, in_=in_), reads, writes)

    def st(self, out, in_, reads):
        ob = Buf("out")
        t = self.P.dma("sp", lambda e: e.dma_start(out=out, in_=in_), reads, [ob])
        self.final.append(t)
        return t

    def dbg(self, name, ap, reads):
        if name not in self.dbg_names:
            return
        shape = list(ap.shape)
        d = self.dout("dbg_" + name, shape, ap.dtype)
        self.st(d, ap, reads)

    def arena_f(self, off, n):
        return self.arena[:, off:off + n]

    def arena_b(self, off, n):
        return self.arena[:, off:off + n].bitcast(BF16)

    def build(self):
        nc, P = self.nc, self.P
        xmain = self.din("xmain", [NT, D])
        xprev = self.din("xprev", [NPR, D])
        cvec_d = self.din("cvec", [128, 8])
        ident_d = self.din("ident", [128, 128])
        lnp = self.din("lnp", [6, D])
        w_in_l = self.din("w_in_l", [56, 128, 16 * 128])
        b_in_d = self.din("b_in_l", [128, 56])
        rgp_d = self.din("rgp", [128, 8 * 8])
        rgw_d = self.din("rg_w", [128, 2 * 8 * 128])
        sth_d = self.din("st_h", [128, 8 * 16])
        stc_d = self.din("st_conv", [128, 8 * 3 * 16])
        sts5_d = self.din("st_s5", [128, 64 * 16])
        s5sc_d = self.din("s5sc", [128, 3 * 64])
        s5b_d = self.din("s5b", [128, 2, 64, 16])
        s5c_d = self.din("s5c", [128, 2, 64, 16])
        s5d_d = self.din("s5d", [128, 8])
        gluw_d = self.din("glu_w_l", [8, 128, 8 * 128])
        glub_d = self.din("glu_b_l", [128, 8])
        pa_d = self.din("proj_a_l", [16, 128, 8 * 128])
        pb_d = self.din("proj_b_l", [16, 128, 8 * 128])
        wo_d = self.din("w_o_l", [4, 128, 16 * 512])
        rw_d = self.din("router_w", [D, NE])
        rb_d = self.din("router_bias", [1, NE])
        w1_d = self.din("ex_w1", [NE, D, 512])
        w3_d = self.din("ex_w3", [NE, D, 512])
        w2_d = self.din("ex_w2", [NE, 512, D])
        sw1_d = self.din("sh_w1", [D, 512])
        sw3_d = self.din("sh_w3", [D, 512])
        sw2_d = self.din("sh_w2", [512, D])
        y_d = self.dout("y", [NT, D])
        rgout_d = self.dout("rgout", [128, 8 * 17])
        convout_d = self.dout("convout", [128, 8 * 51])
        s5out_d = self.dout("s5out", [128, 64 * 17])

        self.arena = self.sb("arena", [128, AW], F32)
        ident = self.sb("ident_s", [128, 128], F32)
        identb = self.sb("identb", [128, 128], BF16)
        cvec = self.sb("cvec_s", [128, 8], F32)
        psum = self.es.enter_context(nc.psum_tensor("psum", [128, 8 * 512], F32))
        bank = [psum[:, i * 512:(i + 1) * 512] for i in range(8)]
        bbank = [Buf(f"bank{i}") for i in range(8)]
        b_ident, b_identb, b_cvec = Buf(), Buf(), Buf()
        self.ld(ident[:], ident_d, [b_ident])
        self.ld(cvec[:], cvec_d, [b_cvec])
        self.I("dve", "tensor_copy", [b_ident], [b_identb], identb[:], ident[:])
        FLAG, SGN, NSGN, MLO, NMHI, NMLO = (cvec[:, i:i + 1] for i in range(6))

        R_A, R_B, R_C, R_D, R_E, R_W = 0, 8384, 16640, 25024, 29216, 33408
        X0T = self.arena_b(R_A, 8384).rearrange("p (k t) -> p k t", k=16)
        X0Tp = self.arena_b(R_B, 8256).rearrange("p (k t) -> p k t", k=16)
        G5 = self.arena_f(R_C, 8384).rearrange("p (k t) -> p k t", k=8)
        G5b = self.arena_b(R_D, 4192).rearrange("p (k t) -> p k t", k=8)
        ACTA = self.arena_b(R_E, 4192).rearrange("p (k t) -> p k t", k=8)
        bX0T = [Buf(f"x0t{i}") for i in range(9)]
        bX0Tp = [Buf(f"x0tp{i}") for i in range(9)]

        so = R_W
        XT = [self.arena_f(so + i * 2048, 2048) for i in range(2)]
        so += 4096
        XB = self.arena_b(so, 1024)
        so += 1024
        LNG = self.arena_f(so, 2048)
        LNB = self.arena_f(so + 2048, 2048)
        so += 4096
        STAT = self.arena_f(so, 32)
        so += 32
        bXT = [Buf("xt0"), Buf("xt1")]
        bXB, bLN, bSTAT = Buf("xb"), Buf("ln"), Buf("stat")
        self.ld(LNG, lnp[0:1, :].partition_broadcast(128), [bLN])
        self.ld(LNB, lnp[1:2, :].partition_broadcast(128), [bLN])

        def ln_tile(src, r0, rows, xt, bxt, lng, lnb, bln, out_ap, bout, eps_done=None):
            self.ld(xt[:rows], src[r0:r0 + rows, :], [bxt])
            st6 = STAT[:, 0:24].rearrange("p (c s) -> p c s", c=4)
            for c in range(4):
                self.I("dve", "bn_stats", [bxt], [bSTAT], st6[:rows, c, :], xt[:rows, c * 512:(c + 1) * 512])
            self.I("dve", "bn_aggr", [bSTAT], [bSTAT], STAT[:rows, 24:26], STAT[:rows, 0:24])
            self.act(STAT[:rows, 26:27], STAT[:rows, 25:26], AF.Sqrt, [bSTAT], [bSTAT], bias=EPS, scale=1.0)
            self.I("dve", "reciprocal", [bSTAT], [bSTAT], STAT[:rows, 27:28], STAT[:rows, 26:27])
            self.I("dve", "tensor_scalar", [bxt, bSTAT], [bxt], xt[:rows], xt[:rows], STAT[:rows, 24:25],
                   STAT[:rows, 27:28], ALU.subtract, ALU.mult)
            self.I("pool", "tensor_tensor", [bxt, bln], [bxt], xt[:rows], xt[:rows], lng[:rows], ALU.mult)
            self.I("dve", "tensor_tensor", [bxt, bln], [bout], out_ap, xt[:rows], lnb[:rows], ALU.add)

        def transpose_tile(xb, bxb, rows, dst3, c0, bdst):
            for h in range(2):
                pb = bank[h].bitcast(BF16)
                for kk in range(8):
                    kc = h * 8 + kk
                    self.tr(pb[:, kk * 128:kk * 128 + rows], xb[:rows, kc::16], identb[:rows, :rows],
                            [bxb, b_identb], [bbank[h]])
                src = pb.rearrange("p (k t) -> p k t", k=8)[:, :, :rows]
                self.act(dst3[:, h * 8:(h + 1) * 8, c0:c0 + rows], src, AF.Copy, [bbank[h]], [bdst])

        for (src, n, dst3, bd) in ((xprev, NPR, X0Tp, bX0Tp), (xmain, NT, X0T, bX0T)):
            for i, (r0, rows) in enumerate(_tiles(n)):
                ln_tile(src, r0, rows, XT[i % 2], bXT[i % 2], LNG, LNB, bLN, XB[:rows], bXB)
                transpose_tile(XB, bXB, rows, dst3, r0, bd[i])
        self.dbg("x0t", X0T, bX0T)
        self.dbg("x0tp", X0Tp, bX0Tp)
        if self.stage <= 1:
            return self.finish()
        P.barrier()

        so = R_W
        WC = [self.arena_b(so + i * 1024, 1024).rearrange("p (k m) -> p k m", k=16) for i in range(2)]
        bWC = [Buf("wc0"), Buf("wc1")]
        so += 2048
        BIN = self.arena_f(so, 56)
        so += 56
        bBIN = Buf("bin")
        self.ld(BIN, b_in_d, [bBIN])
        self.wc_ctr = 0

        def load_wc(c):
            s = self.wc_ctr % 2
            self.wc_ctr += 1
            self.ld(WC[s].rearrange("p k m -> p (k m)"), w_in_l[c], [bWC[s]], q="pool")
            return WC[s], bWC[s]

        def zchunk(wc, bwc, xT, bxT, n, consume, banks=(0, 1, 2)):
            for bi, (s, w) in enumerate(_blocks(n)):
                bk = banks[bi % len(banks)]
                tl = sorted(set(range(s // 128, (s + w - 1) // 128 + 1)))
                rd = [bwc] + [bxT[t] for t in tl]
                for kc in range(16):
                    self.mm(bank[bk][:, :w], wc[:, kc, :], xT[:, kc, s:s + w], kc == 0, kc == 15, rd, [bbank[bk]])
                consume(bi, s, w, bank[bk][:, :w], bbank[bk])

        SC = self.arena_f(so, 24 * 64).rearrange("p (a g) -> p a g", a=24)
        so += 24 * 64
        bSC = Buf("sc")
        S5IN = self.arena_f(so, 3 * 64).rearrange("p (a g) -> p a g", a=3)
        so += 192
        bS5IN = Buf()
        self.ld(S5IN.rearrange("p a g -> p (a g)"), s5sc_d, [bS5IN])
        ARE, AIM, LDT = S5IN[:, 0, :], S5IN[:, 1, :], S5IN[:, 2, :]
        (s_DT, s_RHO, s_TH, s_T, s_R, s_SIN1, s_COS1, s_ABR, s_ABI, s_DEN, s_NR, s_CR, s_CI, s_S2, s_CRN,
         s_T2, s_T3) = range(17)
        sc = lambda i: SC[:, i, :]
        rsc = [bSC, bS5IN, b_cvec]

        def vtt(o, a, b, op):
            self.I("dve", "tensor_tensor", rsc, [bSC], o, a, b, op)

        def vts(o, a, s1, s2, op0, op1=None):
            if op1 is None:
                self.I("dve", "tensor_scalar", rsc, [bSC], o, a, s1, None, op0)
            else:
                self.I("dve", "tensor_scalar", rsc, [bSC], o, a, s1, s2, op0, op1)

        self.act(sc(s_DT), LDT, AF.Exp, rsc, [bSC])
        vtt(sc(s_T), ARE, sc(s_DT), ALU.mult)
        self.act(sc(s_RHO), sc(s_T), AF.Exp, rsc, [bSC])
        vtt(sc(s_TH), AIM, sc(s_DT), ALU.mult)

        def sin_of(dst, shift):
            vts(sc(s_T2), sc(s_TH), shift, None, ALU.add)
            vts(sc(s_T), sc(s_T2), 1.0 / TWO_PI, MAGIC, ALU.mult, ALU.add)
            vts(sc(s_T), sc(s_T), -MAGIC, None, ALU.add)
            self.I("dve", "scalar_tensor_tensor", rsc, [bSC], sc(s_R), sc(s_T), -TWO_PI, sc(s_T2), ALU.mult, ALU.add)
            self.act(dst, sc(s_R), AF.Sin, rsc, [bSC])

        sin_of(sc(s_SIN1), 0.0)
        sin_of(sc(s_COS1), math.pi / 2)
        vtt(sc(s_ABR), sc(s_RHO), sc(s_COS1), ALU.mult)
        vtt(sc(s_ABI), sc(s_RHO), sc(s_SIN1), ALU.mult)
        vtt(sc(s_T), ARE, ARE, ALU.mult)
        vtt(sc(s_T2), AIM, AIM, ALU.mult)
        vtt(sc(s_DEN), sc(s_T), sc(s_T2), ALU.add)
        self.I("dve", "reciprocal", rsc, [bSC], sc(s_DEN), sc(s_DEN))
        vts(sc(s_NR), sc(s_ABR), -1.0, None, ALU.add)
        vtt(sc(s_T), sc(s_NR), ARE, ALU.mult)
        vtt(sc(s_T2), sc(s_ABI), AIM, ALU.mult)
        vtt(sc(s_T), sc(s_T), sc(s_T2), ALU.add)
        vtt(sc(s_CR), sc(s_T), sc(s_DEN), ALU.mult)
        vtt(sc(s_T), sc(s_ABI), ARE, ALU.mult)
        vtt(sc(s_T2), sc(s_NR), AIM, ALU.mult)
        vtt(sc(s_T), sc(s_T), sc(s_T2), ALU.subtract)
        vtt(sc(s_CI), sc(s_T), sc(s_DEN), ALU.mult)
        vts(sc(s_S2), sc(s_CI), SGN, None, ALU.mult)
        vts(sc(s_CRN), sc(s_CR), NSGN, None, ALU.mult)
        self.dbg("sc", SC.rearrange("p a g -> p (a g)"), [bSC])

        self._ranges = [[so, AW], [R_E, R_E + 4192]]

        def take(n):
            for r in self._ranges:
                if r[0] + n <= r[1]:
                    o = r[0]
                    r[0] += n
                    return o
            raise AssertionError(("arena overflow", n, self._ranges))
        HC = self.arena_f(take(64), 64)
        bHC = [Buf(f"hc{g}") for g in range(64)]
        S5OUT = self.arena_f(take(64 * 17), 64 * 17).rearrange("p (g s) -> p g s", g=64)
        bS5OUT = Buf("s5out")
        STS5 = self.arena_f(take(8 * 16), 8 * 16).rearrange("p (g s) -> p g s", g=8)
        bSTS5 = Buf()
        sts5_v = sts5_d.rearrange("p (g s) -> p g s", g=64)
        DL = self.arena_f(take(8), 8)
        bDL = Buf()
        self.ld(DL, s5d_d, [bDL])
        JM = self.arena_f(take(128), 128)
        bJM = Buf()
        self.I("dve", "tensor_copy", [b_ident], [bJM], JM[:, 0:64], ident[:, 64:128])
        self.I("dve", "tensor_copy", [b_ident], [bJM], JM[:, 64:128], ident[:, 0:64])
        self.I("pool", "memset", [], bHC, HC, 0.0)
        BSEL = self.arena_f(take(256), 256).rearrange("p (a g c) -> p a g c", a=2, g=8)
        CSEL = self.arena_f(take(256), 256).rearrange("p (a g c) -> p a g c", a=2, g=8)
        bBSEL, bCSEL = Buf(), Buf()
        BB = self.arena_f(take(512), 512).rearrange("p (a g c) -> p a g c", a=4, g=8)
        bBB = Buf()
        PAD = self.arena_f(take(1024), 1024).rearrange("p (a g m) -> p a g m", a=1, g=8)
        bPAD = Buf()
        LXY = self.arena_b(take(1024), 1024).rearrange("p (a g m) -> p a g m", a=2, g=8)
        bLXY = Buf()
        L12 = self.arena_b(take(1024), 1024).rearrange("p (a g m) -> p a g m", a=2, g=8)
        bL12 = Buf()
        ROT = self.arena_f(take(2048), 2048).rearrange("p (a g m) -> p a g m", a=2, g=8)
        bROT = Buf()
        TLS = self.arena_f(take(16), 16).rearrange("p (a g) -> p a g", a=2)
        JT = self.arena_f(take(128), 128)
        bJT = Buf()
        CS8 = self.arena_f(take(2 * 8 * LCH), 2 * 8 * LCH).rearrange("p (a g t) -> p a g t", a=2, g=8)
        bCS = Buf("cs8")
        TG = self.arena_f(take(8 * 128), 8 * 128).rearrange("p (a g t) -> p a g t", a=1, g=8)
        bTG = Buf()
        USF = self.arena_f(take(NT), NT)
        bUSF = Buf("usf")
        USB = self.arena_b(take(NT // 2), NT // 2)
        bUSB = Buf("usb")
        USBP = self.arena_b(take(NPR // 2), NPR // 2)
        bUSBP = Buf("usbp")
        TT = [self.arena_f(take(2 * LCH), 2 * LCH).rearrange("p (a t) -> p a t", a=2) for _ in range(2)]
        bTT = [Buf(), Buf()]
        MM_ = [self.arena_f(take(LCH), LCH) for _ in range(2)]
        bMM = [Buf(), Buf()]
        GS = [self.arena_f(take(LCH), LCH) for _ in range(2)]
        bGS = [Buf(), Buf()]
        PB = [self.arena_b(take(LCH), LCH).rearrange("p (a t) -> p a t", a=2) for _ in range(2)]
        bPB = [Buf(), Buf()]
        Y5S = self.arena_f(take(LCH), LCH)
        bY5S = Buf()
        self.I("pool", "memset", [], [bPAD], PAD.rearrange("p a g m -> p (a g m)"), 0.0)
        self.I("pool", "memset", [], [bL12], L12.rearrange("p a g m -> p (a g m)"), 0.0)
        bG5 = [Buf(f"g5_{j}") for j in range(8)]
        bG5b = [Buf(f"g5b_{j}") for j in range(8)]

        def diag_dst(t3):
            flat = t3.rearrange("p g m -> p (g m)")
            a = flat[:, 0:1008].rearrange("p (g r) -> p g r", r=144)[:, :, 0:16]
            b = flat[:, 1008:1024].rearrange("p (g r) -> p g r", g=1)
            return [(a, 0, 7), (b, 7, 8)]

        gctr = 0
        for j in range(8):
            gs = slice(8 * j, 8 * j + 8)
            self.ld(BSEL, s5b_d[:, :, gs, :], [bBSEL])
            self.ld(CSEL, s5c_d[:, :, gs, :], [bCSEL])
            self.ld(STS5, sts5_v[:, gs, :], [bSTS5])
            rd = [bBSEL, bSC, bBB]
            for (dst, s_a, s_b, ia, ib) in ((0, s_CR, s_S2, 0, 1), (1, s_CI, s_CRN, 0, 1)):
                self.I("dve", "tensor_tensor", rd, [bBB], BB[:, 2], BSEL[:, ia], b3(SC[:, s_a, gs], 16), ALU.mult)
                self.I("dve", "tensor_tensor", rd, [bBB], BB[:, 3], BSEL[:, ib], b3(SC[:, s_b, gs], 16), ALU.mult)
                self.I("dve", "tensor_tensor", rd, [bBB], BB[:, dst], BB[:, 2], BB[:, 3], ALU.add)
            for a in range(2):
                for (dst, g0, g1) in diag_dst(PAD[:, 0]):
                    self.I("dve", "tensor_copy", [bBB], [bPAD], dst, BB[:, a, g0:g1, :])
                for hh in range(2):
                    bk = 3 + hh
                    for q in range(4):
                        gq = hh * 4 + q
                        self.tr(bank[bk][:, q * 128:(q + 1) * 128], PAD[:, 0, gq, :], ident[:], [bPAD, b_ident], [bbank[bk]])
                    self.act(LXY[:, a, hh * 4:(hh + 1) * 4, :], bank[bk].rearrange("p (g m) -> p g m", g=4), AF.Copy,
                             [bbank[bk]], [bLXY])
            for (a, i0, m0, i1, m1) in ((0, 0, MLO, 1, NMHI), (1, 1, NMLO, 0, NMHI)):
                self.I("dve", "tensor_scalar", [bCSEL, b_cvec], [bBB], BB[:, 2], CSEL[:, i0], m0, None, ALU.mult)
                self.I("dve", "scalar_tensor_tensor", [bCSEL, b_cvec, bBB], [bBB], BB[:, 3], CSEL[:, i1], m1, BB[:, 2],
                       ALU.mult, ALU.add)
                for (dst, g0, g1) in diag_dst(L12[:, a]):
                    self.I("dve", "tensor_copy", [bBB], [bL12], dst, BB[:, 3, g0:g1, :])
            self.I("dve", "tensor_copy", [bSC], [bCS], CS8[:, 0, :, 0:1], SC[:, s_COS1, gs].rearrange("p (g o) -> p g o", o=1))
            self.I("dve", "tensor_copy", [bSC], [bCS], CS8[:, 1, :, 0:1], SC[:, s_SIN1, gs].rearrange("p (g o) -> p g o", o=1))
            n = 1
            while n < LCH:
                w = min(n, LCH - n)
                ck = CS8[:, 0, :, n - 1:n].to_broadcast([128, 8, w])
                sk = CS8[:, 1, :, n - 1:n].to_broadcast([128, 8, w])
                co, si = CS8[:, 0, :, 0:w], CS8[:, 1, :, 0:w]
                cn, sn = CS8[:, 0, :, n:n + w], CS8[:, 1, :, n:n + w]
                tt_ = TG[:, 0, :, 0:w]
                rdt = [bCS, bTG]
                self.I("dve", "tensor_tensor", rdt, [bCS], cn, co, ck, ALU.mult)
                self.I("dve", "tensor_tensor", rdt, [bTG], tt_, si, sk, ALU.mult)
                self.I("dve", "tensor_tensor", rdt, [bCS], cn, cn, tt_, ALU.subtract)
                self.I("dve", "tensor_tensor", rdt, [bCS], sn, si, ck, ALU.mult)
                self.I("dve", "tensor_tensor", rdt, [bTG], tt_, co, sk, ALU.mult)
                self.I("dve", "tensor_tensor", rdt, [bCS], sn, sn, tt_, ALU.add)
                n += w
            self.I("dve", "tensor_scalar", [bCS, b_cvec], [bJT], TLS[:, 0, :], CS8[:, 1, :, LCH - 1], NSGN, None, ALU.mult)
            self.I("dve", "tensor_scalar", [bCS, b_cvec], [bJT], TLS[:, 1, :], CS8[:, 1, :, 0], NSGN, None, ALU.mult)
            for a, col in ((0, LCH - 1), (1, 0)):
                for g in range(8):
                    self.I("dve", "tensor_scalar", [bJM, bJT], [bJT], JT, JM, TLS[:, a, g:g + 1], None, ALU.mult)
                    self.I("dve", "scalar_tensor_tensor", [b_ident, bCS, bJT], [bROT], ROT[:, a, g, :], ident[:],
                           CS8[:, 0, g, col:col + 1], JT, ALU.mult, ALU.add)
            if j == 0:
                self.dbg("lxy", LXY.rearrange("p a g m -> p (a g m)"), [bLXY])
                self.dbg("l12", L12.rearrange("p a g m -> p (a g m)"), [bL12])
                self.dbg("cs8", CS8.rearrange("p a g t -> p (a g t)"), [bCS])
                self.dbg("rot", ROT.rearrange("p a g m -> p (a g m)"), [bROT])

            wc, bwc = load_wc(16 + j)

            def cons_p(bi, s, w, ps, bps):
                self.act(USBP[:, s:s + w], ps, AF.Identity, [bps, bBIN], [bUSBP], bias=BIN[:, 16 + j:17 + j], scale=1.0)
            zchunk(wc, bwc, X0Tp, bX0Tp, NPR, cons_p)

            def cons_m(bi, s, w, ps, bps):
                self.act(USF[:, s:s + w], ps, AF.Identity, [bps, bBIN], [bUSF], bias=BIN[:, 16 + j:17 + j], scale=1.0)
            zchunk(wc, bwc, X0T, bX0T, NT, cons_m)
            self.I("pool", "tensor_copy", [bUSF], [bUSB], USB, USF)
            if j == 0:
                self.dbg("usf", USF, [bUSF])

            its = [(ip, k, g) for ip in (0, 1) for k in range(4) for g in range(8)] + [(1, 4, g) for g in range(8)]

            def geom(it):
                ip, k, g = it
                samp = k == 4
                c0, wd = (k * LCH, LCH) if not samp else (NPR, NS)
                return ip, k, g, samp, c0, wd

            def s0(n, it):
                ip, k, g, samp, c0, wd = geom(it)
                xyb = n % 2
                src, bsrc = (USB, bUSB) if ip == 1 else (USBP, bUSBP)
                Xp = psum[:, (2 * xyb) * 512:(2 * xyb) * 512 + wd]
                Yp = psum[:, (2 * xyb + 1) * 512:(2 * xyb + 1) * 512 + wd]
                self.mm(Xp, LXY[:, 0, g, :], src[:, c0:c0 + wd], True, True, [bLXY, bsrc], [bbank[2 * xyb]])
                self.mm(Yp, LXY[:, 1, g, :], src[:, c0:c0 + wd], True, True, [bLXY, bsrc], [bbank[2 * xyb + 1]])

            def s1(n, it):
                ip, k, g, samp, c0, wd = geom(it)
                xyb = n % 2
                XYv = psum[:, (2 * xyb) * 512:(2 * xyb + 2) * 512].rearrange("p (a t) -> p a t", a=2)[:, :, 0:wd]
                cs = CS8[:, :, g, :] if not samp else CS8[:, :, g, 0:1].to_broadcast([128, 2, NS])
                self.I("dve", "tensor_tensor", [bbank[2 * xyb], bbank[2 * xyb + 1], bCS], [bTT[n % 2]], TT[n % 2][:, :, 0:wd], XYv, cs, ALU.mult)

            def s2(n, it):
                ip, k, g, samp, c0, wd = geom(it)
                self.I("pool", "tensor_tensor", [bTT[n % 2]], [bMM[n % 2]], MM_[n % 2][:, 0:wd], TT[n % 2][:, 0, 0:wd], TT[n % 2][:, 1, 0:wd], ALU.add)

            def s3(n, it):
                ip, k, g, samp, c0, wd = geom(it)
                main = ip == 1
                gg = 8 * j + g
                gsb = n % 2
                mm_, bmm = MM_[n % 2], bMM[n % 2]
                if not samp:
                    self.I("dve", "tensor_tensor_scan", [bmm, bSC, bHC[gg]], [bGS[gsb]], GS[gsb],
                           SC[:, s_RHO, gg:gg + 1].to_broadcast([128, LCH]), mm_, HC[:, gg:gg + 1], ALU.mult, ALU.add)
                    self.mm(bank[5][:, 0:1], ROT[:, 0, g, :], GS[gsb][:, LCH - 1:LCH], True, True, [bROT, bGS[gsb]], [bbank[5]])
                    if (not main) and k == 3:
                        self.I("dve", "tensor_scalar", [bbank[5], b_cvec], [bHC[gg]], HC[:, gg:gg + 1], bank[5][:, 0:1], FLAG, None, ALU.mult)
                    elif main and k == 3:
                        self.act(S5OUT[:, gg, 0:1], bank[5][:, 0:1], AF.Copy, [bbank[5]], [bS5OUT])
                    else:
                        self.act(HC[:, gg:gg + 1], bank[5][:, 0:1], AF.Copy, [bbank[5]], [bHC[gg]])
                else:
                    self.I("dve", "scalar_tensor_tensor", [bmm, bSC, bSTS5], [bGS[gsb]], GS[gsb][:, 0:NS], STS5[:, g, :],
                           SC[:, s_RHO, gg:gg + 1], mm_[:, 0:NS], ALU.mult, ALU.add)
                    self.mm(bank[5][:, 0:NS], ROT[:, 1, g, :], GS[gsb][:, 0:NS], True, True, [bROT, bGS[gsb]], [bbank[5]])
                    self.act(S5OUT[:, gg, 1:17], bank[5][:, 0:NS], AF.Copy, [bbank[5]], [bS5OUT])
                if main:
                    cs = CS8[:, :, g, :] if not samp else CS8[:, :, g, 0:1].to_broadcast([128, 2, NS])
                    self.I("pool", "tensor_tensor", [bCS, bGS[gsb]], [bPB[gsb]], PB[gsb][:, :, 0:wd], cs,
                           GS[gsb][:, 0:wd].rearrange("p (o t) -> p o t", o=1).to_broadcast([128, 2, wd]), ALU.mult)

            def s5(n, it):
                ip, k, g, samp, c0, wd = geom(it)
                if ip != 1:
                    return
                gsb = n % 2
                ybk = 6 + (k % 2)
                self.mm(bank[ybk][:, 0:wd], L12[:, 0, g, :], PB[gsb][:, 0, 0:wd], g == 0, False, [bL12, bPB[gsb]], [bbank[ybk]])
                self.mm(bank[ybk][:, 0:wd], L12[:, 1, g, :], PB[gsb][:, 1, 0:wd], False, g == 7, [bL12, bPB[gsb]], [bbank[ybk]])
                if g == 7:
                    self.I("dve", "scalar_tensor_tensor", [bUSF, bDL, bbank[ybk]], [bY5S], Y5S[:, 0:wd], USF[:, c0:c0 + wd],
                           DL[:, j:j + 1], bank[ybk][:, 0:wd], ALU.mult, ALU.add)
                    self.act(G5[:, j, c0:c0 + wd], Y5S[:, 0:wd], AF.Gelu_apprx_tanh, [bY5S], [bG5[j]])
                    self.I("pool", "tensor_copy", [bG5[j]], [bG5b[j]], G5b[:, j, c0:c0 + wd], G5[:, j, c0:c0 + wd])

            NI = len(its)
            s0(0, its[0])
            s0(1, its[1])
            s1(0, its[0])
            s2(0, its[0])
            for n in range(NI):
                if n + 2 < NI:
                    s0(n + 2, its[n + 2])
                if n + 1 < NI:
                    s1(n + 1, its[n + 1])
                    s2(n + 1, its[n + 1])
                s3(n, its[n])
                if n >= 1:
                    s5(n - 1, its[n - 1])
            s5(NI - 1, its[NI - 1])
        self.st(s5out_d, S5OUT.rearrange("p g s -> p (g s)"), [bS5OUT])
        self.dbg("g5", G5.rearrange("p k t -> p (k t)"), bG5)
        if self.stage <= 2:
            return self.finish()
        P.barrier()

        self._ranges = [[R_W + 2104, AW]]
        RGP = self.arena_f(take(64), 64).rearrange("p (h k) -> p h k", h=8)
        bRGP = Buf()
        self.ld(RGP.rearrange("p h k -> p (h k)"), rgp_d, [bRGP])
        GW = self.arena_b(take(1024), 1024).rearrange("p (a h j) -> p a h j", a=2, h=8)
        bGW = Buf()
        self.ld(GW.rearrange("p a h j -> p (a h j)"), rgw_d, [bGW], q="pool")
        STH = self.arena_f(take(128), 128).rearrange("p (h s) -> p h s", h=8)
        STC = self.arena_f(take(384), 384).rearrange("p (h k s) -> p h k s", h=8, k=3)
        bSTH, bSTC = Buf(), Buf()
        self.ld(STH.rearrange("p h s -> p (h s)"), sth_d, [bSTH])
        self.ld(STC.rearrange("p h k s -> p (h k s)"), stc_d, [bSTC])
        RGOUT = self.arena_f(take(136), 136).rearrange("p (h s) -> p h s", h=8)
        CONVOUT = self.arena_f(take(408), 408).rearrange("p (h s) -> p h s", h=8)
        bRGOUT, bCONVOUT = Buf(), Buf()
        C8 = self.arena_f(take(16), 16)
        bC8 = Buf()
        HL = self.arena_f(take(8), 8)
        bHL = Buf()
        XAp = self.arena_f(take(NPR + 3), NPR + 3)
        XA = self.arena_f(take(NT + 3), NT + 3)
        XC = self.arena_f(take(NT), NT)
        XCb = self.arena_b(take(NT // 2), NT // 2)
        Rb_, Ib_, Ab_, Sb_, Hb_, YG = (self.arena_f(take(NT), NT) for _ in range(6))
        bXAp, bXA, bXC, bXCb, bR, bI, bA, bS, bH, bYG = (Buf() for _ in range(10))
        bACTA = [Buf() for _ in range(8)]
        self.act(C8[:, 0:8], RGP[:, :, 7], AF.Exp, [bRGP], [bC8], scale=-1.0)
        self.act(C8[:, 0:8], C8[:, 0:8], AF.Ln, [bC8], [bC8], bias=1.0, scale=1.0)
        self.I("dve", "tensor_scalar", [bC8], [bC8], C8[:, 0:8], C8[:, 0:8], -8.0, None, ALU.mult)
        self.I("dve", "tensor_scalar", [bC8], [bC8], C8[:, 8:16], C8[:, 0:8], 2.0, None, ALU.mult)
        self.I("pool", "memset", [], [bXAp], XAp[:, 0:3], 0.0)

        passes = ((X0Tp, bX0Tp, NPR, XAp, bXAp), (X0T, bX0T, NT, XA, bXA))

        def rg_front(h, ip):
            xT, bxT, n_tok, XAx, bXAx = passes[ip]
            wc, bwc = load_wc(h)

            def cons(bi_, s, w, ps, bps):
                self.act(XAx[:, 3 + s:3 + s + w], ps, AF.Identity, [bps, bBIN], [bXAx], bias=BIN[:, h:h + 1], scale=1.0)
            zchunk(wc, bwc, xT, bxT, n_tok, cons)
            if ip == 1:
                wc2, bwc2 = load_wc(8 + h)

                def cons2(bi_, s, w, ps, bps):
                    self.act(YG[:, s:s + w], ps, AF.Gelu_apprx_tanh, [bps, bBIN], [bYG], bias=BIN[:, 8 + h:9 + h], scale=1.0)
                zchunk(wc2, bwc2, X0T, bX0T, NT, cons2)

        def rg_back(h, ip):
            xT, bxT, n_tok, XAx, bXAx = passes[ip]
            main = ip == 1
            wk = [RGP[:, h, k:k + 1] for k in range(4)]
            cb, ba, bi = RGP[:, h, 4:5], RGP[:, h, 5:6], RGP[:, h, 6:7]
            self.I("dve", "tensor_scalar", [bXAx, bRGP], [bXC], XC[:, 0:NPR], XAx[:, 0:NPR], wk[0], cb, ALU.mult, ALU.add)
            for k in range(1, 4):
                self.I("dve", "scalar_tensor_tensor", [bXAx, bRGP, bXC], [bXC], XC[:, 0:NPR], XAx[:, k:k + NPR], wk[k],
                       XC[:, 0:NPR], ALU.mult, ALU.add)
            if main:
                xs_ = XC[:, NPR:NT]
                self.I("dve", "tensor_scalar", [bSTC, bRGP], [bXC], xs_, STC[:, h, 0, :], wk[0], cb, ALU.mult, ALU.add)
                for k in (1, 2):
                    self.I("dve", "scalar_tensor_tensor", [bSTC, bRGP, bXC], [bXC], xs_, STC[:, h, k, :], wk[k], xs_, ALU.mult, ALU.add)
                self.I("dve", "scalar_tensor_tensor", [bXA, bRGP, bXC], [bXC], xs_, XA[:, 3 + NPR:3 + NT], wk[3], xs_, ALU.mult, ALU.add)
            self.act(XCb[:, :n_tok], XC[:, :n_tok], AF.Copy, [bXC], [bXCb])
            for (which, dstb, bdst, bias_) in ((0, Rb_, bR, ba), (1, Ib_, bI, bi)):
                for bi_, (s, w) in enumerate(_blocks(n_tok)):
                    bk = 3 + (bi_ % 2) + 2 * which
                    self.mm(bank[bk][:, :w], GW[:, which, h, :], XCb[:, s:s + w], True, True, [bGW, bXCb], [bbank[bk]])
                    self.act(dstb[:, s:s + w], bank[bk][:, :w], AF.Sigmoid, [bbank[bk], bRGP], [bdst], bias=bias_, scale=1.0)
            self.act(Ab_[:, :n_tok], Rb_[:, :n_tok], AF.Exp, [bR, bC8], [bA], scale=C8[:, h:h + 1])
            self.act(Sb_[:, :n_tok], Rb_[:, :n_tok], AF.Exp, [bR, bC8], [bS], scale=C8[:, 8 + h:9 + h])
            self.act(Sb_[:, :n_tok], Sb_[:, :n_tok], AF.Sqrt, [bS], [bS], bias=1.0, scale=-1.0)
            self.I("dve", "tensor_tensor", [bI, bXC], [bI], Ib_[:, :n_tok], Ib_[:, :n_tok], XC[:, :n_tok], ALU.mult)
            self.I("dve", "tensor_tensor", [bI, bS], [bI], Ib_[:, :n_tok], Ib_[:, :n_tok], Sb_[:, :n_tok], ALU.mult)
            if not main:
                self.I("dve", "tensor_tensor_scan", [bA, bI], [bH], Hb_[:, 0:NPR], Ab_[:, 0:NPR], Ib_[:, 0:NPR], 0.0, ALU.mult, ALU.add)
                self.I("dve", "tensor_scalar", [bH, b_cvec], [bHL], HL[:, h:h + 1], Hb_[:, NPR - 1:NPR], FLAG, None, ALU.mult)
                self.I("dve", "tensor_scalar", [bXAp, b_cvec], [bXA], XA[:, 0:3], XAp[:, NPR:NPR + 3], FLAG, None, ALU.mult)
            else:
                self.I("dve", "tensor_tensor_scan", [bA, bI, bHL], [bH], Hb_[:, 0:NPR], Ab_[:, 0:NPR], Ib_[:, 0:NPR], HL[:, h:h + 1],
                       ALU.mult, ALU.add)
                self.I("dve", "tensor_tensor", [bSTH, bA], [bH], Hb_[:, NPR:NT], STH[:, h, :], Ab_[:, NPR:NT], ALU.mult)
                self.I("dve", "tensor_tensor", [bH, bI], [bH], Hb_[:, NPR:NT], Hb_[:, NPR:NT], Ib_[:, NPR:NT], ALU.add)
                self.act(RGOUT[:, h, :], Hb_[:, NPR - 1:NT], AF.Copy, [bH], [bRGOUT])
                self.act(CONVOUT[:, h, 0:3], XA[:, NPR:NPR + 3], AF.Copy, [bXA], [bCONVOUT])
                cv = CONVOUT[:, h, 3:51].rearrange("p (s k) -> p s k", k=3)
                self.act(cv[:, :, 0], STC[:, h, 1, :], AF.Copy, [bSTC], [bCONVOUT])
                self.act(cv[:, :, 1], STC[:, h, 2, :], AF.Copy, [bSTC], [bCONVOUT])
                self.act(cv[:, :, 2], XA[:, 3 + NPR:3 + NT], AF.Copy, [bXA], [bCONVOUT])
                self.I("dve", "tensor_tensor", [bH, bYG], [bACTA[h]], ACTA[:, h, :], Hb_, YG, ALU.mult)

        rg_its = [(h, ip) for h in range(8) for ip in (0, 1)]
        rg_front(*rg_its[0])
        for i, it in enumerate(rg_its):
            if i + 1 < len(rg_its):
                rg_front(*rg_its[i + 1])
            rg_back(*it)
        self.st(rgout_d, RGOUT.rearrange("p h s -> p (h s)"), [bRGOUT])
        self.st(convout_d, CONVOUT.rearrange("p h s -> p (h s)"), [bCONVOUT])
        self.dbg("acta", ACTA.rearrange("p k t -> p (k t)"), bACTA)
        if self.stage <= 3:
            return self.finish()
        P.barrier()

        self._ranges = [[R_W + 2104, AW]]
        GLUb = self.arena_b(R_B, 4192).rearrange("p (k t) -> p k t", k=8)
        bGLUb = [Buf() for _ in range(8)]
        GWT = [self.arena_b(take(512), 512).rearrange("p (k m) -> p k m", k=8) for _ in range(2)]
        bGWT = [Buf(), Buf()]
        SG = [self.arena_f(take(512), 512) for _ in range(2)]
        bSG = [Buf(), Buf()]
        GLB = self.arena_f(take(8), 8)
        bGLB = Buf()
        self.ld(GLB, glub_d, [bGLB])
        ctr = 0
        for n in range(8):
            gw, bgw = GWT[n % 2], bGWT[n % 2]
            self.ld(gw.rearrange("p k m -> p (k m)"), gluw_d[n], [bgw], q="pool")
            for bi_, (s, w) in enumerate(_blocks(NT)):
                bk = ctr % 4
                sg, bsg = SG[ctr % 2], bSG[ctr % 2]
                ctr += 1
                for kc in range(8):
                    self.mm(bank[bk][:, :w], gw[:, kc, :], G5b[:, kc, s:s + w], kc == 0, kc == 7, [bgw, bG5b[kc]], [bbank[bk]])
                self.act(sg[:, :w], bank[bk][:, :w], AF.Sigmoid, [bbank[bk], bGLB], [bsg], bias=GLB[:, n:n + 1], scale=1.0)
                self.I("dve", "tensor_tensor", [bG5[n], bsg], [bGLUb[n]], GLUb[:, n, s:s + w], G5[:, n, s:s + w], sg[:, :w], ALU.mult)
        self.dbg("glub", GLUb.rearrange("p k t -> p (k t)"), bGLUb)
        if self.stage <= 4:
            return self.finish()
        P.barrier()

        self._ranges = [[R_W + 2104, AW]]
        MT = self.arena_b(R_C, 8384).rearrange("p (k t) -> p k t", k=16)
        bMT = [Buf() for _ in range(16)]
        PAW = [self.arena_b(take(512), 512).rearrange("p (k m) -> p k m", k=8) for _ in range(2)]
        PBW = [self.arena_b(take(512), 512).rearrange("p (k m) -> p k m", k=8) for _ in range(2)]
        bPAW, bPBW = [Buf(), Buf()], [Buf(), Buf()]
        WG = [self.arena_b(take(1024), 1024).rearrange("p (k m) -> p k m", k=16) for _ in range(4)]
        bWG = [Buf() for _ in range(4)]
        T1 = [self.arena_f(take(512), 512) for _ in range(2)]
        T2 = [self.arena_f(take(512), 512) for _ in range(2)]
        bT1, bT2 = [Buf(), Buf()], [Buf(), Buf()]
        ctr = 0
        for m in range(16):
            pa, bpa, pb, bpb = PAW[m % 2], bPAW[m % 2], PBW[m % 2], bPBW[m % 2]
            wga, bwga, wgb, bwgb = WG[2 * (m % 2)], bWG[2 * (m % 2)], WG[2 * (m % 2) + 1], bWG[2 * (m % 2) + 1]
            self.ld(pa.rearrange("p k m -> p (k m)"), pa_d[m], [bpa], q="pool")
            self.ld(pb.rearrange("p k m -> p (k m)"), pb_d[m], [bpb], q="pool")
            self.ld(wga.rearrange("p k m -> p (k m)"), w_in_l[24 + m], [bwga], q="pool")
            self.ld(wgb.rearrange("p k m -> p (k m)"), w_in_l[40 + m], [bwgb], q="pool")
            for bi_, (s, w) in enumerate(_blocks(NT)):
                par = ctr % 2
                ctr += 1
                kA, kB, kGA, kGB = 4 * par, 4 * par + 1, 4 * par + 2, 4 * par + 3
                tl = sorted(set(range(s // 128, (s + w - 1) // 128 + 1)))
                rx = [bX0T[t] for t in tl]
                for kc in range(8):
                    self.mm(bank[kA][:, :w], pa[:, kc, :], ACTA[:, kc, s:s + w], kc == 0, kc == 7, [bpa, bACTA[kc]], [bbank[kA]])
                for kc in range(8):
                    self.mm(bank[kB][:, :w], pb[:, kc, :], GLUb[:, kc, s:s + w], kc == 0, kc == 7, [bpb, bGLUb[kc]], [bbank[kB]])
                for kc in range(16):
                    self.mm(bank[kGA][:, :w], wga[:, kc, :], X0T[:, kc, s:s + w], kc == 0, kc == 15, [bwga] + rx, [bbank[kGA]])
                for kc in range(16):
                    self.mm(bank[kGB][:, :w], wgb[:, kc, :], X0T[:, kc, s:s + w], kc == 0, kc == 15, [bwgb] + rx, [bbank[kGB]])
                t1, bt1, t2, bt2 = T1[par], bT1[par], T2[par], bT2[par]
                self.act(t1[:, :w], bank[kGA][:, :w], AF.Sigmoid, [bbank[kGA], bBIN], [bt1], bias=BIN[:, 24 + m:25 + m], scale=1.0)
                self.act(t2[:, :w], bank[kGB][:, :w], AF.Sigmoid, [bbank[kGB], bBIN], [bt2], bias=BIN[:, 40 + m:41 + m], scale=1.0)
                self.I("dve", "tensor_tensor", [bt1, bbank[kA]], [bt1], t1[:, :w], t1[:, :w], bank[kA][:, :w], ALU.mult)
                self.I("dve", "tensor_tensor", [bt2, bbank[kB]], [bt2], t2[:, :w], t2[:, :w], bank[kB][:, :w], ALU.mult)
                self.I("pool", "tensor_tensor", [bt1, bt2], [bMT[m]], MT[:, m, s:s + w], t1[:, :w], t2[:, :w], ALU.add)
        self.dbg("mt", MT.rearrange("p k t -> p (k t)"), bMT)
        if self.stage <= 5:
            return self.finish()
        P.barrier()

        TILES = _tiles(NT)
        X1 = [self.arena_f(R_D + i * 2048, 2048) for i in range(9)]
        bX1 = [Buf(f"x1_{i}") for i in range(9)]
        X1T = self.arena_b(R_A, 8384).rearrange("p (k t) -> p k t", k=16)
        bX1T = [Buf(f"x1t{i}") for i in range(9)]
        WO = self.arena_b(R_B, 4096).rearrange("p (k n) -> p k n", k=16)
        bWO = Buf()
        LNG1 = self.arena_f(R_B + 4096, 2048)
        LNB1 = self.arena_f(R_B + 6144, 2048)
        bLN1 = Buf()
        so = R_D + 9 * 2048
        XT1 = self.arena_f(so, 2048)
        bXT1 = Buf()
        LNG0 = self.arena_f(so + 2048, 2048)
        LNB0 = self.arena_f(so + 4096, 2048)
        bLN0 = Buf()
        XB = self.arena_b(so + 6144, 1024)
        bXB = Buf()
        STAT = self.arena_f(so + 7168, 32)
        bSTAT = Buf()
        assert so + 7200 <= AW
        self.ld(LNG0, lnp[0:1, :].partition_broadcast(128), [bLN0])
        self.ld(LNB0, lnp[1:2, :].partition_broadcast(128), [bLN0])
        self.ld(LNG1, lnp[2:3, :].partition_broadcast(128), [bLN1])
        self.ld(LNB1, lnp[3:4, :].partition_broadcast(128), [bLN1])
        for f in range(4):
            self.ld(WO.rearrange("p k n -> p (k n)").rearrange("p (a b) -> p a b", a=4),
                    wo_d[f].rearrange("p (a b) -> p a b", a=4), [bWO], q="pool")
            for i, (r0, rows) in enumerate(TILES):
                bk = i % 4
                for m in range(16):
                    self.mm(bank[bk][:rows, :], MT[:, m, r0:r0 + rows], WO[:, m, :], m == 0, m == 15, [bMT[m], bWO], [bbank[bk]])
                self.act(X1[i][:rows, f * 512:(f + 1) * 512], bank[bk][:rows, :], AF.Copy, [bbank[bk]], [bX1[i]])

        def ln_inplace(x, bx, rows, lng, lnb, bln, STAT=STAT, bSTAT=bSTAT):
            st6 = STAT[:, 0:24].rearrange("p (c s) -> p c s", c=4)
            for c in range(4):
                self.I("dve", "bn_stats", [bx], [bSTAT], st6[:rows, c, :], x[:rows, c * 512:(c + 1) * 512])
            self.I("dve", "bn_aggr", [bSTAT], [bSTAT], STAT[:rows, 24:26], STAT[:rows, 0:24])
            self.act(STAT[:rows, 26:27], STAT[:rows, 25:26], AF.Sqrt, [bSTAT], [bSTAT], bias=EPS, scale=1.0)
            self.I("dve", "reciprocal", [bSTAT], [bSTAT], STAT[:rows, 27:28], STAT[:rows, 26:27])
            self.I("dve", "tensor_scalar", [bx, bSTAT], [bx], x[:rows], x[:rows], STAT[:rows, 24:25],
                   STAT[:rows, 27:28], ALU.subtract, ALU.mult)
            self.I("pool", "tensor_tensor", [bx, bln], [bx], x[:rows], x[:rows], lng[:rows], ALU.mult)
            self.I("dve", "tensor_tensor", [bx, bln], [bx], x[:rows], x[:rows], lnb[:rows], ALU.add)
        self.ln_inplace = ln_inplace

        XT1s = [XT1, self.arena_f(R_C, 2048)]
        bXT1s = [bXT1, Buf()]
        XBs = [XB, self.arena_b(R_C + 2048, 1024)]
        bXBs = [bXB, Buf()]
        for i, (r0, rows) in enumerate(TILES):
            xt1, bxt1 = XT1s[i % 2], bXT1s[i % 2]
            extra = list(bMT) if i == 1 else []
            self.P.dma("sp", lambda e, xt1=xt1, r0=r0, rows=rows: e.dma_start(out=xt1[:rows], in_=xmain[r0:r0 + rows, :]),
                       [], [bxt1] + extra, chan=bxt1)
            ln_inplace(xt1, bxt1, rows, LNG0, LNB0, bLN0)
            self.I("dve", "scalar_tensor_tensor", [bxt1, bX1[i]], [bX1[i]], X1[i][:rows], xt1[:rows], ALPHA, X1[i][:rows], ALU.mult, ALU.add)
            ln_inplace(X1[i], bX1[i], rows, LNG1, LNB1, bLN1)
            xb_, bxb_ = XBs[i % 2], bXBs[i % 2]
            wr = [bxb_] + (list(bMT) if i == 1 else [])
            self.act(xb_[:rows], X1[i][:rows], AF.Copy, [bX1[i]], wr)
            transpose_tile(xb_, bxb_, rows, X1T, r0, bX1T[i])
        for i, (r0, rows) in enumerate(TILES):
            self.dbg(f"x1_{i}", X1[i][:rows], [bX1[i]])
        if self.stage <= 6:
            return self.finish()
        P.barrier()

        AX = mybir.AxisListType
        so = R_D + 9 * 2048
        self._ranges = [[so, AW]]
        SL = [self.arena_b(R_B + q * 4096, 4096) for q in range(4)]
        bSL = [Buf(f"slot{q}") for q in range(4)]
        Hh = [self.arena_b(take(2096), 2096).rearrange("p (k t) -> p k t", k=4) for _ in range(2)]
        bHh = [[Buf() for _ in range(3)] for _ in range(2)]
        Ss = [self.arena_f(take(512), 512) for _ in range(2)]
        bSs = [Buf(), Buf()]
        GATES = [self.arena_f(take(NE + 1), NE + 1) for _ in range(9)]
        bGATES = [Buf(f"gates{i}") for i in range(9)]
        RW = self.arena_b(take(512), 512).rearrange("p (k n) -> p k n", k=16)
        bRW = Buf()
        RB = self.arena_f(take(64), 64)
        bRB = Buf()
        STAT2 = self.arena_f(take(32), 32)
        RS = self.arena_f(take(64 * 5 + 8 * 5 + 8), 64 * 5 + 48)
        bRS = Buf("rs")
        SCO, SELB, GTOP, MASKED, EM = (RS[:, i * 64:(i + 1) * 64] for i in range(5))
        GSC, GS8, GMASK, PEN, TOP8, WS = (RS[:, 320 + i * 8:328 + i * 8] for i in range(6))
        self.ld(RW.rearrange("p k n -> p (k n)"), rw_d.rearrange("(p kc) n -> p (kc n)", kc=16), [bRW], q="pool")
        self.ld(RB, rb_d.partition_broadcast(128), [bRB])
        for i in range(9):
            self.I("pool", "memset", [], [bGATES[i]], GATES[i][:, NE:NE + 1], 1.0)

        def wsrc(e, which):
            if e < NE:
                d = (w1_d, w3_d, w2_d)[which][e]
            else:
                d = (sw1_d, sw3_d, sw2_d)[which]
            if which < 2:
                return d.rearrange("(p kc) n -> p (kc n)", kc=16).rearrange("p (a b) -> p a b", a=4)
            return d.rearrange("(jc p) n -> p jc n", p=128)

        NEX = NE + 1

        def load_w(idx):
            e, which = divmod(idx, 3)
            if e >= NEX:
                return
            q = idx % 4
            self.ld(SL[q].rearrange("p (a b) -> p a b", a=4), wsrc(e, which), [bSL[q]], q="pool")

        for idx in range(4):
            load_w(idx)

        SELB3 = SELB.rearrange("p (g k) -> p g k", g=8)
        GTOP3 = GTOP.rearrange("p (g k) -> p g k", g=8)
        MASKED3 = MASKED.rearrange("p (g k) -> p g k", g=8)
        rr = [bRS]
        def router_tile(i):
            r0, rows = TILES[i]
            bk = 4 + (i % 2)
            for kc in range(16):
                self.mm(bank[bk][:rows, 0:64], X1T[:, kc, r0:r0 + rows], RW[:, kc, :], kc == 0, kc == 15, [bX1T[i], bRW], [bbank[bk]])
            self.I("pool", "tensor_scalar", [bX1[i]], [bX1[i]], X1[i][:rows], X1[i][:rows], ALPHA, None, ALU.mult)
            self.act(SCO[:rows], bank[bk][:rows, 0:64], AF.Sigmoid, [bbank[bk]] + rr, rr)
            self.I("dve", "tensor_tensor", rr + [bRB], rr, SELB[:rows], SCO[:rows], RB[:rows], ALU.add)
            for g in range(8):
                self.I("dve", "max", rr, rr, GTOP[:rows, g * 8:(g + 1) * 8], SELB[:rows, g * 8:(g + 1) * 8])
            self.I("dve", "tensor_tensor", rr, rr, GSC[:rows], GTOP3[:rows, :, 0], GTOP3[:rows, :, 1], ALU.add)
            self.I("dve", "max", rr, rr, GS8[:rows], GSC[:rows])
            self.I("dve", "tensor_scalar", rr, rr, GMASK[:rows], GSC[:rows], GS8[:rows, 3:4], None, ALU.is_ge)
            self.I("dve", "tensor_scalar", rr, rr, PEN[:rows], GMASK[:rows], -1.0, 1e30, ALU.add, ALU.mult)
            self.I("dve", "tensor_tensor", rr, rr, MASKED3[:rows], SELB3[:rows], b3(GMASK[:rows], 8), ALU.mult)
            self.I("dve", "tensor_tensor", rr, rr, MASKED3[:rows], MASKED3[:rows], b3(PEN[:rows], 8), ALU.add)
            self.I("dve", "max", rr, rr, TOP8[:rows], MASKED[:rows])
            self.I("dve", "tensor_scalar", rr, rr, EM[:rows], MASKED[:rows], TOP8[:rows, 7:8], None, ALU.is_ge)
            self.I("dve", "tensor_tensor", rr, rr, EM[:rows], EM[:rows], SCO[:rows], ALU.mult)
            self.I("dve", "reduce_sum", rr, rr, WS[:rows, 0:1], EM[:rows], AX.X)
            self.I("dve", "reciprocal", rr, rr, WS[:rows, 1:2], WS[:rows, 0:1])
            self.I("dve", "tensor_scalar", rr, [bGATES[i]], GATES[i][:rows, 0:NE], EM[:rows], WS[:rows, 1:2], 2.5, ALU.mult, ALU.mult)

        self._rt_next = 0
        BLK = [(0, 384), (384, 384), (768, 280)]
        blk_tiles = {0: [0, 1, 2], 1: [3, 4, 5], 2: [6, 7, 8]}
        octr = 0
        for e in range(NEX):
            par = e % 2
            q1, q3, q2 = (3 * e) % 4, (3 * e + 1) % 4, (3 * e + 2) % 4
            W1 = SL[q1].rearrange("p (k n) -> p k n", k=16)
            W3 = SL[q3].rearrange("p (k n) -> p k n", k=16)
            W2 = SL[q2].rearrange("p (k n) -> p k n", k=4)

            def ab(bi):
                s, w = BLK[bi]
                tl = blk_tiles[bi]
                rx = [bX1T[t] for t in tl]
                for jc in range(4):
                    ka, kb = jc % 2, 2 + (jc % 2)
                    for kc in range(16):
                        self.mm(bank[ka][:, :w], W1[:, kc, jc * 128:(jc + 1) * 128], X1T[:, kc, s:s + w], kc == 0, kc == 15,
                                [bSL[q1]] + rx, [bbank[ka]])
                    for kc in range(16):
                        self.mm(bank[kb][:, :w], W3[:, kc, jc * 128:(jc + 1) * 128], X1T[:, kc, s:s + w], kc == 0, kc == 15,
                                [bSL[q3]] + rx, [bbank[kb]])
                    ss_, bss = Ss[jc % 2], bSs[jc % 2]
                    self.act(ss_[:, :w], bank[ka][:, :w], AF.Silu, [bbank[ka]], [bss])
                    self.I("dve", "tensor_tensor", [bss, bbank[kb]], [bHh[par][bi]], Hh[par][:, jc, s:s + w], ss_[:, :w],
                           bank[kb][:, :w], ALU.mult)
                    if e == 0 and self._rt_next < 9:
                        router_tile(self._rt_next)
                        self._rt_next += 1

            def w2p(bi):
                nonlocal octr
                for t in blk_tiles[bi]:
                    r0, rows = TILES[t]
                    for f in range(4):
                        ok = 4 + (octr % 4)
                        octr += 1
                        for jc in range(4):
                            self.mm(bank[ok][:rows, :], Hh[par][:, jc, r0:r0 + rows], W2[:, jc, f * 512:(f + 1) * 512], jc == 0, jc == 3,
                                    [bHh[par][bi], bSL[q2]], [bbank[ok]])
                        self.I("dve", "scalar_tensor_tensor", [bX1[t], bbank[ok], bGATES[t]], [bX1[t]], X1[t][:rows, f * 512:(f + 1) * 512],
                               bank[ok][:rows, :], GATES[t][:rows, e:e + 1], X1[t][:rows, f * 512:(f + 1) * 512], ALU.mult, ALU.add)

            ab(0)
            ab(1)
            w2p(0)
            ab(2)
            load_w(3 * e + 4)
            load_w(3 * e + 5)
            w2p(1)
            w2p(2)
            load_w(3 * e + 6)
        if self.stage <= 7:
            self.dbg("acc0", X1[0], [bX1[0]])
        P.barrier()

        LNG2 = self.arena_f(R_B, 2048)
        LNB2 = self.arena_f(R_B + 2048, 2048)
        bLN2 = Buf()
        self.ld(LNG2, lnp[4:5, :].partition_broadcast(128), [bLN2])
        self.ld(LNB2, lnp[5:6, :].partition_broadcast(128), [bLN2])
        bST2 = Buf()
        for i, (r0, rows) in enumerate(TILES):
            ln_inplace(X1[i], bX1[i], rows, LNG2, LNB2, bLN2, STAT2, bST2)
            self.st(y_d[r0:r0 + rows, :], X1[i][:rows], [bX1[i]])
        return self.finish()

    def finish(self):
        self.P.emit(self.final)
        return self.nc


def _c(a):
    return np.ascontiguousarray(a, dtype=np.float32)


def prep_inputs(inp):
    g = {k: np.asarray(v) for k, v in inp.items()}
    xp = g["x_prompt"]
    meta = g["meta_tokens"]
    xs = g["x_sample"][:, 0, :]
    p = np.arange(128)
    cvec = np.zeros((128, 8), np.float32)
    cvec[:, 1] = np.where(p < 64, -1.0, 1.0)
    cvec[:, 2] = -cvec[:, 1]
    cvec[:, 3] = (p < 64)
    cvec[:, 4] = -(p >= 64).astype(np.float32)
    cvec[:, 5] = -(p < 64).astype(np.float32)
    shared = {}
    shared["ident"] = np.eye(128, dtype=np.float32)
    shared["lnp"] = _c(np.stack([g["ln_in_g"], g["ln_in_b"], g["ln1_g"][0], g["ln1_b"][0], g["ln2_g"][0], g["ln2_b"][0]]))
    w_in = g["w_in"][0]
    shared["w_in_l"] = _c(w_in.reshape(128, 16, 56, 128).transpose(2, 0, 1, 3).reshape(56, 128, 2048))
    shared["b_in_l"] = _c(g["b_in"][0].reshape(56, 128).T)
    rgp = np.zeros((128, 8, 8), np.float32)
    cw = g["conv_w"][0].reshape(4, 8, 128)
    for k in range(4):
        rgp[:, :, k] = cw[k].T
    rgp[:, :, 4] = g["conv_b"][0].reshape(8, 128).T
    rgp[:, :, 5] = g["rg_ba"][0].reshape(8, 128).T
    rgp[:, :, 6] = g["rg_bi"][0].reshape(8, 128).T
    rgp[:, :, 7] = g["rg_lambda"][0].reshape(8, 128).T
    shared["rgp"] = _c(rgp.reshape(128, 64))
    rgw = np.stack([g["rg_wa"][0].transpose(1, 0, 2), g["rg_wi"][0].transpose(1, 0, 2)], axis=1)
    shared["rg_w"] = _c(rgw.reshape(128, 2 * 8 * 128))
    two = lambda a: np.concatenate([a, a], axis=0)
    shared["s5sc"] = _c(np.stack([two(g["s5_a_re"][0].T), two(g["s5_a_im"][0].T),
                                  np.broadcast_to(g["s5_log_dt"][0][None, :], (128, 64))], axis=1).reshape(128, 192))
    bre = g["s5_b_re"][0].transpose(1, 0, 2)
    bim = g["s5_b_im"][0].transpose(1, 0, 2)
    shared["s5b"] = _c(np.stack([np.concatenate([bre, bim], 0), np.concatenate([bim, bre], 0)], axis=1))
    cre = g["s5_c_re"][0].transpose(2, 0, 1)
    cim = g["s5_c_im"][0].transpose(2, 0, 1)
    shared["s5c"] = _c(np.stack([two(cre), two(cim)], axis=1))
    shared["s5d"] = _c(g["s5_d"][0].reshape(8, 128).T)
    shared["glu_w_l"] = _c(g["glu_w"][0].reshape(8, 128, 8, 128).transpose(2, 1, 0, 3).reshape(8, 128, 1024))
    shared["glu_b_l"] = _c(g["glu_b"][0].reshape(8, 128).T)
    shared["proj_a_l"] = _c(g["proj_a"][0].reshape(8, 128, 16, 128).transpose(2, 1, 0, 3).reshape(16, 128, 1024))
    shared["proj_b_l"] = _c(g["proj_b"][0].reshape(8, 128, 16, 128).transpose(2, 1, 0, 3).reshape(16, 128, 1024))
    shared["w_o_l"] = _c(g["w_o"][0].reshape(16, 128, 4, 512).transpose(2, 1, 0, 3).reshape(4, 128, 16 * 512))
    shared["router_w"] = _c(g["router_w"][0])
    shared["router_bias"] = _c(g["router_bias"][0][None, :])
    shared["ex_w1"] = _c(g["ex_w1"][0])
    shared["ex_w3"] = _c(g["ex_w3"][0])
    shared["ex_w2"] = _c(g["ex_w2"][0])
    shared["sh_w1"] = _c(g["sh_w1"][0])
    shared["sh_w3"] = _c(g["sh_w3"][0])
    shared["sh_w2"] = _c(g["sh_w2"][0])
    maps = []
    for c in range(8):
        b, half = c // 2, c % 2
        full = np.concatenate([meta, xp[b]], axis=0)
        m = dict(shared)
        own = full[half * NPR:(half + 1) * NPR]
        ss = slice(c * NS, (c + 1) * NS)
        m["xmain"] = _c(np.concatenate([own, xs[ss]], axis=0))
        m["xprev"] = _c(full[0:NPR]) if half == 1 else np.zeros((NPR, D), np.float32)
        cv = cvec.copy()
        cv[:, 0] = float(half)
        m["cvec"] = cv
        sh = g["state_rglru_h"][0][ss]
        m["st_h"] = _c(sh.reshape(NS, 8, 128).transpose(2, 1, 0).reshape(128, 128))
        sc = g["state_conv"][0][ss]
        m["st_conv"] = _c(sc.reshape(NS, 3, 8, 128).transpose(3, 2, 1, 0).reshape(128, 8 * 3 * 16))
        sr = g["state_s5_re"][0][ss].transpose(2, 1, 0)
        si = g["state_s5_im"][0][ss].transpose(2, 1, 0)
        m["st_s5"] = _c(np.concatenate([sr, si], axis=0).reshape(128, 64 * 16))
        maps.append(m)
    return maps


_NC_CACHE = {}


def _get_nc():
    if "nc" not in _NC_CACHE:
        k = K()
        _NC_CACHE["nc"] = k.build()
    return _NC_CACHE["nc"]


def kernel(**inputs):
    maps = prep_inputs(inputs)
    nc = _get_nc()
    res = run_bass_kernel_spmd(nc, maps, core_ids=list(range(8)))
    R = res.results
    f32 = np.float32
    y_prompt = np.zeros((4, 2048, D), f32)
    y_sample = np.zeros((128, 1, D), f32)
    p_h = np.zeros((1, 4, DR), f32)
    p_c = np.zeros((1, 4, 3, DR), f32)
    p_r = np.zeros((1, 4, 64, 64), f32)
    p_i = np.zeros((1, 4, 64, 64), f32)
    s_h = np.zeros((1, 128, DR), f32)
    s_c = np.zeros((1, 128, 3, DR), f32)
    s_r = np.zeros((1, 128, 64, 64), f32)
    s_i = np.zeros((1, 128, 64, 64), f32)
    for c in range(8):
        b, half = c // 2, c % 2
        y = np.asarray(R[c]["y"])
        if half == 0:
            y_prompt[b, 0:NPR - 16] = y[16:NPR]
        else:
            y_prompt[b, NPR - 16:2048] = y[0:NPR]
        ss = slice(c * NS, (c + 1) * NS)
        y_sample[ss, 0] = y[NPR:NT]
        rg = np.asarray(R[c]["rgout"]).reshape(128, 8, 17)
        cv = np.asarray(R[c]["convout"]).reshape(128, 8, 51)
        s5 = np.asarray(R[c]["s5out"]).reshape(128, 64, 17)
        s_h[0, ss] = rg[:, :, 1:].transpose(2, 1, 0).reshape(NS, DR)
        s_c[0, ss] = cv[:, :, 3:].reshape(128, 8, NS, 3).transpose(2, 3, 1, 0).reshape(NS, 3, DR)
        s_r[0, ss] = s5[:64, :, 1:].transpose(2, 1, 0)
        s_i[0, ss] = s5[64:, :, 1:].transpose(2, 1, 0)
        if half == 1:
            p_h[0, b] = rg[:, :, 0].T.reshape(DR)
            p_c[0, b] = cv[:, :, 0:3].transpose(2, 1, 0).reshape(3, DR)
            p_r[0, b] = s5[:64, :, 0].T
            p_i[0, b] = s5[64:, :, 0].T
    return (y_prompt, y_sample, p_h, p_c, p_r, p_i, s_h, s_c, s_r, s_i)
```

```python
import math
from contextlib import ExitStack

import numpy as np
import concourse.bass as bass
import concourse.mybir as mybir
from concourse.bass_utils import run_bass_kernel_spmd

F32 = mybir.dt.float32
BF16 = mybir.dt.bfloat16
AF = mybir.ActivationFunctionType
ALU = mybir.AluOpType

D = 2048
NT = 1048
NPR = 1032
NS = 16
LCH = 258
DR = 1024
NE = 64
ALPHA = 2.0 ** 0.25
EPS = 1e-5
MAGIC = 12582912.0
TWO_PI = 2.0 * math.pi
AW = 51200

ENGS = ("pe", "act", "dve", "pool", "sp")


class Buf:
    __slots__ = ("name", "w", "r", "sem", "semval")

    def __init__(self, name=""):
        self.name = name
        self.w = None
        self.r = []
        self.sem = None
        self.semval = 0


class Prog:
    def __init__(self, nc):
        self.nc = nc
        self.ops = {e: [] for e in ENGS}
        self.esem = {}
        self.nsem = 0
        self.floor = []
        self.dma_toks = []

    def _newsem(self, name):
        self.nsem += 1
        return self.nc.alloc_semaphore(name=f"{name}_{self.nsem}")

    def _deps(self, eng, reads, writes):
        deps = list(self.floor)
        for b in reads:
            if b.w is not None:
                deps.append(b.w)
        for b in writes:
            if b.w is not None:
                deps.append(b.w)
            deps.extend(b.r)
        out = []
        seen = set()
        for t in deps:
            if t in seen:
                continue
            seen.add(t)
            if t[0] == "e" and t[1] == "pe" and eng == "pe":
                continue
            out.append(t)
        for t in out:
            if t[0] == "e":
                self.ops[t[1]][t[2]]["sig"] = True
        return out

    def op(self, eng, fn, reads=(), writes=()):
        deps = self._deps(eng, reads, writes)
        idx = len(self.ops[eng])
        self.ops[eng].append({"fn": fn, "deps": deps, "sig": False, "dma": None})
        tok = ("e", eng, idx)
        for b in reads:
            b.r.append(tok)
        for b in writes:
            b.w = tok
            b.r = []
        return tok

    def dma(self, q, fn, reads=(), writes=(), chan=None):
        deps = self._deps(q, reads, writes)
        if chan is None:
            chan = writes[0]
        if chan.sem is None:
            chan.sem = self._newsem("d")
        chan.semval += 16
        tok = ("d", chan.sem, chan.semval)
        self.ops[q].append({"fn": fn, "deps": deps, "sig": False, "dma": (chan.sem, 16)})
        for b in reads:
            b.r.append(tok)
        for b in writes:
            b.w = tok
            b.r = []
        self.dma_toks.append(tok)
        return tok

    def barrier(self):
        fl = []
        for e in ("pe", "act", "dve", "pool"):
            if self.ops[e]:
                for i in range(len(self.ops[e]) - 1, -1, -1):
                    if self.ops[e][i]["dma"] is None:
                        self.ops[e][i]["sig"] = True
                        fl.append(("e", e, i))
                        break
        fl.extend(self.dma_toks)
        self.dma_toks = []
        self.floor = fl

    def emit(self, final_tokens=()):
        nc = self.nc
        for e in ("pe", "act", "dve", "pool"):
            self.esem[e] = self._newsem("e" + e)
        cnt = {}
        for e in ENGS:
            c = 0
            arr = []
            for o in self.ops[e]:
                if o["sig"]:
                    c += 1
                arr.append(c)
            cnt[e] = arr
        prog = self

        def run(e, eng):
            known = {}
            for o in prog.ops[e]:
                for t in o["deps"]:
                    if t[0] == "e":
                        v = cnt[t[1]][t[2]]
                        key = ("e", t[1])
                        if known.get(key, 0) >= v:
                            continue
                        known[key] = v
                        eng.wait_ge(prog.esem[t[1]], v)
                    else:
                        key = ("d", id(t[1]))
                        if known.get(key, 0) >= t[2]:
                            continue
                        known[key] = t[2]
                        eng.wait_ge(t[1], t[2])
                ins = o["fn"](eng)
                if o["dma"] is not None:
                    ins.then_inc(o["dma"][0], o["dma"][1])
                elif o["sig"]:
                    ins.then_inc(prog.esem[e], 1)
            if e == "sp":
                for t in final_tokens:
                    if t[0] == "d":
                        eng.wait_ge(t[1], t[2])
                    else:
                        eng.wait_ge(prog.esem[t[1]], cnt[t[1]][t[2]])

        with nc.Block() as block:
            @block.sync
            def _(eng):
                run("sp", eng)

            @block.tensor
            def _(eng):
                run("pe", eng)

            @block.scalar
            def _(eng):
                run("act", eng)

            @block.vector
            def _(eng):
                run("dve", eng)

            @block.gpsimd
            def _(eng):
                run("pool", eng)


def _blocks(n):
    out = []
    s = 0
    while s < n:
        w = min(512, n - s)
        out.append((s, w))
        s += w
    return out


def _tiles(n):
    out = []
    s = 0
    while s < n:
        w = min(128, n - s)
        out.append((s, w))
        s += w
    return out


def b3(ap2, n):
    P_, a = ap2.shape
    return ap2.rearrange("p (a o) -> p a o", o=1).to_broadcast([P_, a, n])


class K:
    def __init__(self, stage=99, dbg=()):
        self.stage = stage
        self.dbg_names = dbg
        self.nc = bass.Bass("TRN2", target_bir_lowering=False)
        self.P = Prog(self.nc)
        self.es = ExitStack()
        self.final = []
        self.dbg_specs = {}

    def din(self, name, shape):
        return self.nc.dram_tensor(name, list(shape), F32, kind="ExternalInput").ap()

    def dout(self, name, shape, dt=F32):
        return self.nc.dram_tensor(name, list(shape), dt, kind="ExternalOutput").ap()

    def sb(self, name, shape, dt=F32):
        return self.es.enter_context(self.nc.sbuf_tensor(name, list(shape), dt))

    def I(self, eng, name, reads, writes, *a, **kw):
        return self.P.op(eng, lambda e: getattr(e, name)(*a, **kw), reads, writes)

    def act(self, out, in_, func, reads, writes, bias=None, scale=None):
        kw = {}
        if bias is not None:
            kw["bias"] = bias
        if scale is not None:
            kw["scale"] = scale
        return self.P.op("act", lambda e: e.activation(out, in_, func, **kw), reads, writes)

    def mm(self, out, lhsT, rhs, start, stop, reads, writes):
        return self.P.op("pe", lambda e: e.matmul(out, lhsT, rhs, start=start, stop=stop), reads, writes)

    def tr(self, out, in_, ident, reads, writes):
        return self.P.op("pe", lambda e: e.transpose(out, in_, ident), reads, writes)

    def ld(self, out, in_, writes, reads=(), q="sp"):
        return self.P.dma(q, lambda e: e.dma_start(out=out, in_=in_), reads, writes)

    def st(self, out, in_, reads):
        ob = Buf("out")
        t = self.P.dma("sp", lambda e: e.dma_start(out=out, in_=in_), reads, [ob])
        self.final.append(t)
        return t

    def dbg(self, name, ap, reads):
        if name not in self.dbg_names:
            return
        shape = list(ap.shape)
        d = self.dout("dbg_" + name, shape, ap.dtype)
        self.st(d, ap, reads)

    def arena_f(self, off, n):
        return self.arena[:, off:off + n]

    def arena_b(self, off, n):
        return self.arena[:, off:off + n].bitcast(BF16)

    def build(self):
        nc, P = self.nc, self.P
        xmain = self.din("xmain", [NT, D])
        xprev = self.din("xprev", [NPR, D])
        cvec_d = self.din("cvec", [128, 8])
        ident_d = self.din("ident", [128, 128])
        lnp = self.din("lnp", [6, D])
        w_in_l = self.din("w_in_l", [56, 128, 16 * 128])
        b_in_d = self.din("b_in_l", [128, 56])
        rgp_d = self.din("rgp", [128, 8 * 8])
        rgw_d = self.din("rg_w", [128, 2 * 8 * 128])
        sth_d = self.din("st_h", [128, 8 * 16])
        stc_d = self.din("st_conv", [128, 8 * 3 * 16])
        sts5_d = self.din("st_s5", [128, 64 * 16])
        s5sc_d = self.din("s5sc", [128, 3 * 64])
        s5b_d = self.din("s5b", [128, 2, 64, 16])
        s5c_d = self.din("s5c", [128, 2, 64, 16])
        s5d_d = self.din("s5d", [128, 8])
        gluw_d = self.din("glu_w_l", [8, 128, 8 * 128])
        glub_d = self.din("glu_b_l", [128, 8])
        pa_d = self.din("proj_a_l", [16, 128, 8 * 128])
        pb_d = self.din("proj_b_l", [16, 128, 8 * 128])
        wo_d = self.din("w_o_l", [4, 128, 16 * 512])
        rw_d = self.din("router_w", [D, NE])
        rb_d = self.din("router_bias", [1, NE])
        w1_d = self.din("ex_w1", [NE, D, 512])
        w3_d = self.din("ex_w3", [NE, D, 512])
        w2_d = self.din("ex_w2", [NE, 512, D])
        sw1_d = self.din("sh_w1", [D, 512])
        sw3_d = self.din("sh_w3", [D, 512])
        sw2_d = self.din("sh_w2", [512, D])
        y_d = self.dout("y", [NT, D])
        rgout_d = self.dout("rgout", [128, 8 * 17])
        convout_d = self.dout("convout", [128, 8 * 51])
        s5out_d = self.dout("s5out", [128, 64 * 17])

        self.arena = self.sb("arena", [128, AW], F32)
        ident = self.sb("ident_s", [128, 128], F32)
        identb = self.sb("identb", [128, 128], BF16)
        cvec = self.sb("cvec_s", [128, 8], F32)
        psum = self.es.enter_context(nc.psum_tensor("psum", [128, 8 * 512], F32))
        bank = [psum[:, i * 512:(i + 1) * 512] for i in range(8)]
        bbank = [Buf(f"bank{i}") for i in range(8)]
        b_ident, b_identb, b_cvec = Buf(), Buf(), Buf()
        self.ld(ident[:], ident_d, [b_ident])
        self.ld(cvec[:], cvec_d, [b_cvec])
        self.I("dve", "tensor_copy", [b_ident], [b_identb], identb[:], ident[:])
        FLAG, SGN, NSGN, MLO, NMHI, NMLO = (cvec[:, i:i + 1] for i in range(6))

        R_A, R_B, R_C, R_D, R_E, R_W = 0, 8384, 16640, 25024, 29216, 33408
        X0T = self.arena_b(R_A, 8384).rearrange("p (k t) -> p k t", k=16)
        X0Tp = self.arena_b(R_B, 8256).rearrange("p (k t) -> p k t", k=16)
        G5 = self.arena_f(R_C, 8384).rearrange("p (k t) -> p k t", k=8)
        G5b = self.arena_b(R_D, 4192).rearrange("p (k t) -> p k t", k=8)
        ACTA = self.arena_b(R_E, 4192).rearrange("p (k t) -> p k t", k=8)
        bX0T = [Buf(f"x0t{i}") for i in range(9)]
        bX0Tp = [Buf(f"x0tp{i}") for i in range(9)]

        so = R_W
        XT = [self.arena_f(so + i * 2048, 2048) for i in range(2)]
        so += 4096
        XB = self.arena_b(so, 1024)
        so += 1024
        LNG = self.arena_f(so, 2048)
        LNB = self.arena_f(so + 2048, 2048)
        so += 4096
        STAT = self.arena_f(so, 32)
        so += 32
        bXT = [Buf("xt0"), Buf("xt1")]
        bXB, bLN, bSTAT = Buf("xb"), Buf("ln"), Buf("stat")
        self.ld(LNG, lnp[0:1, :].partition_broadcast(128), [bLN])
        self.ld(LNB, lnp[1:2, :].partition_broadcast(128), [bLN])

        def ln_tile(src, r0, rows, xt, bxt, lng, lnb, bln, out_ap, bout, eps_done=None):
            self.ld(xt[:rows], src[r0:r0 + rows, :], [bxt])
            st6 = STAT[:, 0:24].rearrange("p (c s) -> p c s", c=4)
            for c in range(4):
                self.I("dve", "bn_stats", [bxt], [bSTAT], st6[:rows, c, :], xt[:rows, c * 512:(c + 1) * 512])
            self.I("dve", "bn_aggr", [bSTAT], [bSTAT], STAT[:rows, 24:26], STAT[:rows, 0:24])
            self.act(STAT[:rows, 26:27], STAT[:rows, 25:26], AF.Sqrt, [bSTAT], [bSTAT], bias=EPS, scale=1.0)
            self.I("dve", "reciprocal", [bSTAT], [bSTAT], STAT[:rows, 27:28], STAT[:rows, 26:27])
            self.I("dve", "tensor_scalar", [bxt, bSTAT], [bxt], xt[:rows], xt[:rows], STAT[:rows, 24:25],
                   STAT[:rows, 27:28], ALU.subtract, ALU.mult)
            self.I("pool", "tensor_tensor", [bxt, bln], [bxt], xt[:rows], xt[:rows], lng[:rows], ALU.mult)
            self.I("dve", "tensor_tensor", [bxt, bln], [bout], out_ap, xt[:rows], lnb[:rows], ALU.add)

        def transpose_tile(xb, bxb, rows, dst3, c0, bdst):
            for h in range(2):
                pb = bank[h].bitcast(BF16)
                for kk in range(8):
                    kc = h * 8 + kk
                    self.tr(pb[:, kk * 128:kk * 128 + rows], xb[:rows, kc::16], identb[:rows, :rows],
                            [bxb, b_identb], [bbank[h]])
                src = pb.rearrange("p (k t) -> p k t", k=8)[:, :, :rows]
                self.act(dst3[:, h * 8:(h + 1) * 8, c0:c0 + rows], src, AF.Copy, [bbank[h]], [bdst])

        for (src, n, dst3, bd) in ((xprev, NPR, X0Tp, bX0Tp), (xmain, NT, X0T, bX0T)):
            for i, (r0, rows) in enumerate(_tiles(n)):
                ln_tile(src, r0, rows, XT[i % 2], bXT[i % 2], LNG, LNB, bLN, XB[:rows], bXB)
                transpose_tile(XB, bXB, rows, dst3, r0, bd[i])
        self.dbg("x0t", X0T, bX0T)
        self.dbg("x0tp", X0Tp, bX0Tp)
        if self.stage <= 1:
            return self.finish()
        P.barrier()

        so = R_W
        WC = [self.arena_b(so + i * 1024, 1024).rearrange("p (k m) -> p k m", k=16) for i in range(2)]
        bWC = [Buf("wc0"), Buf("wc1")]
        so += 2048
        BIN = self.arena_f(so, 56)
        so += 56
        bBIN = Buf("bin")
        self.ld(BIN, b_in_d, [bBIN])
        self.wc_ctr = 0

        def load_wc(c):
            s = self.wc_ctr % 2
            self.wc_ctr += 1
            self.ld(WC[s].rearrange("p k m -> p (k m)"), w_in_l[c], [bWC[s]], q="pool")
            return WC[s], bWC[s]

        def zchunk(wc, bwc, xT, bxT, n, consume, banks=(0, 1, 2)):
            for bi, (s, w) in enumerate(_blocks(n)):
                bk = banks[bi % len(banks)]
                tl = sorted(set(range(s // 128, (s + w - 1) // 128 + 1)))
                rd = [bwc] + [bxT[t] for t in tl]
                for kc in range(16):
                    self.mm(bank[bk][:, :w], wc[:, kc, :], xT[:, kc, s:s + w], kc == 0, kc == 15, rd, [bbank[bk]])
                consume(bi, s, w, bank[bk][:, :w], bbank[bk])

        SC = self.arena_f(so, 24 * 64).rearrange("p (a g) -> p a g", a=24)
        so += 24 * 64
        bSC = Buf("sc")
        S5IN = self.arena_f(so, 3 * 64).rearrange("p (a g) -> p a g", a=3)
        so += 192
        bS5IN = Buf()
        self.ld(S5IN.rearrange("p a g -> p (a g)"), s5sc_d, [bS5IN])
        ARE, AIM, LDT = S5IN[:, 0, :], S5IN[:, 1, :], S5IN[:, 2, :]
        (s_DT, s_RHO, s_TH, s_T, s_R, s_SIN1, s_COS1, s_ABR, s_ABI, s_DEN, s_NR, s_CR, s_CI, s_S2, s_CRN,
         s_T2, s_T3) = range(17)
        sc = lambda i: SC[:, i, :]
        rsc = [bSC, bS5IN, b_cvec]

        def vtt(o, a, b, op):
            self.I("dve", "tensor_tensor", rsc, [bSC], o, a, b, op)

        def vts(o, a, s1, s2, op0, op1=None):
            if op1 is None:
                self.I("dve", "tensor_scalar", rsc, [bSC], o, a, s1, None, op0)
            else:
                self.I("dve", "tensor_scalar", rsc, [bSC], o, a, s1, s2, op0, op1)

        self.act(sc(s_DT), LDT, AF.Exp, rsc, [bSC])
        vtt(sc(s_T), ARE, sc(s_DT), ALU.mult)
        self.act(sc(s_RHO), sc(s_T), AF.Exp, rsc, [bSC])
        vtt(sc(s_TH), AIM, sc(s_DT), ALU.mult)

        def sin_of(dst, shift):
            vts(sc(s_T2), sc(s_TH), shift, None, ALU.add)
            vts(sc(s_T), sc(s_T2), 1.0 / TWO_PI, MAGIC, ALU.mult, ALU.add)
            vts(sc(s_T), sc(s_T), -MAGIC, None, ALU.add)
            self.I("dve", "scalar_tensor_tensor", rsc, [bSC], sc(s_R), sc(s_T), -TWO_PI, sc(s_T2), ALU.mult, ALU.add)
            self.act(dst, sc(s_R), AF.Sin, rsc, [bSC])

        sin_of(sc(s_SIN1), 0.0)
        sin_of(sc(s_COS1), math.pi / 2)
        vtt(sc(s_ABR), sc(s_RHO), sc(s_COS1), ALU.mult)
        vtt(sc(s_ABI), sc(s_RHO), sc(s_SIN1), ALU.mult)
        vtt(sc(s_T), ARE, ARE, ALU.mult)
        vtt(sc(s_T2), AIM, AIM, ALU.mult)
        vtt(sc(s_DEN), sc(s_T), sc(s_T2), ALU.add)
        self.I("dve", "reciprocal", rsc, [bSC], sc(s_DEN), sc(s_DEN))
        vts(sc(s_NR), sc(s_ABR), -1.0, None, ALU.add)
        vtt(sc(s_T), sc(s_NR), ARE, ALU.mult)
        vtt(sc(s_T2), sc(s_ABI), AIM, ALU.mult)
        vtt(sc(s_T), sc(s_T), sc(s_T2), ALU.add)
        vtt(sc(s_CR), sc(s_T), sc(s_DEN), ALU.mult)
        vtt(sc(s_T), sc(s_ABI), ARE, ALU.mult)
        vtt(sc(s_T2), sc(s_NR), AIM, ALU.mult)
        vtt(sc(s_T), sc(s_T), sc(s_T2), ALU.subtract)
        vtt(sc(s_CI), sc(s_T), sc(s_DEN), ALU.mult)
        vts(sc(s_S2), sc(s_CI), SGN, None, ALU.mult)
        vts(sc(s_CRN), sc(s_CR), NSGN, None, ALU.mult)
        self.dbg("sc", SC.rearrange("p a g -> p (a g)"), [bSC])

        self._ranges = [[so, AW], [R_E, R_E + 4192]]

        def take(n):
            for r in self._ranges:
                if r[0] + n <= r[1]:
                    o = r[0]
                    r[0] += n
                    return o
            raise AssertionError(("arena overflow", n, self._ranges))
        HC = self.arena_f(take(64), 64)
        bHC = [Buf(f"hc{g}") for g in range(64)]
        S5OUT = self.arena_f(take(64 * 17), 64 * 17).rearrange("p (g s) -> p g s", g=64)
        bS5OUT = Buf("s5out")
        STS5 = self.arena_f(take(8 * 16), 8 * 16).rearrange("p (g s) -> p g s", g=8)
        bSTS5 = Buf()
        sts5_v = sts5_d.rearrange("p (g s) -> p g s", g=64)
        DL = self.arena_f(take(8), 8)
        bDL = Buf()
        self.ld(DL, s5d_d, [bDL])
        JM = self.arena_f(take(128), 128)
        bJM = Buf()
        self.I("dve", "tensor_copy", [b_ident], [bJM], JM[:, 0:64], ident[:, 64:128])
        self.I("dve", "tensor_copy", [b_ident], [bJM], JM[:, 64:128], ident[:, 0:64])
        self.I("pool", "memset", [], bHC, HC, 0.0)
        BSEL = self.arena_f(take(256), 256).rearrange("p (a g c) -> p a g c", a=2, g=8)
        CSEL = self.arena_f(take(256), 256).rearrange("p (a g c) -> p a g c", a=2, g=8)
        bBSEL, bCSEL = Buf(), Buf()
        BB = self.arena_f(take(512), 512).rearrange("p (a g c) -> p a g c", a=4, g=8)
        bBB = Buf()
        PAD = self.arena_f(take(1024), 1024).rearrange("p (a g m) -> p a g m", a=1, g=8)
        bPAD = Buf()
        LXY = self.arena_b(take(1024), 1024).rearrange("p (a g m) -> p a g m", a=2, g=8)
        bLXY = Buf()
        L12 = self.arena_b(take(1024), 1024).rearrange("p (a g m) -> p a g m", a=2, g=8)
        bL12 = Buf()
        ROT = self.arena_f(take(2048), 2048).rearrange("p (a g m) -> p a g m", a=2, g=8)
        bROT = Buf()
        TLS = self.arena_f(take(16), 16).rearrange("p (a g) -> p a g", a=2)
        JT = self.arena_f(take(128), 128)
        bJT = Buf()
        CS8 = self.arena_f(take(2 * 8 * LCH), 2 * 8 * LCH).rearrange("p (a g t) -> p a g t", a=2, g=8)
        bCS = Buf("cs8")
        TG = self.arena_f(take(8 * 128), 8 * 128).rearrange("p (a g t) -> p a g t", a=1, g=8)
        bTG = Buf()
        USF = self.arena_f(take(NT), NT)
        bUSF = Buf("usf")
        USB = self.arena_b(take(NT // 2), NT // 2)
        bUSB = Buf("usb")
        USBP = self.arena_b(take(NPR // 2), NPR // 2)
        bUSBP = Buf("usbp")
        TT = [self.arena_f(take(2 * LCH), 2 * LCH).rearrange("p (a t) -> p a t", a=2) for _ in range(2)]
        bTT = [Buf(), Buf()]
        MM_ = [self.arena_f(take(LCH), LCH) for _ in range(2)]
        bMM = [Buf(), Buf()]
        GS = [self.arena_f(take(LCH), LCH) for _ in range(2)]
        bGS = [Buf(), Buf()]
        PB = [self.arena_b(take(LCH), LCH).rearrange("p (a t) -> p a t", a=2) for _ in range(2)]
        bPB = [Buf(), Buf()]
        Y5S = self.arena_f(take(LCH), LCH)
        bY5S = Buf()
        self.I("pool", "memset", [], [bPAD], PAD.rearrange("p a g m -> p (a g m)"), 0.0)
        self.I("pool", "memset", [], [bL12], L12.rearrange("p a g m -> p (a g m)"), 0.0)
        bG5 = [Buf(f"g5_{j}") for j in range(8)]
        bG5b = [Buf(f"g5b_{j}") for j in range(8)]

        def diag_dst(t3):
            flat = t3.rearrange("p g m -> p (g m)")
            a = flat[:, 0:1008].rearrange("p (g r) -> p g r", r=144)[:, :, 0:16]
            b = flat[:, 1008:1024].rearrange("p (g r) -> p g r", g=1)
            return [(a, 0, 7), (b, 7, 8)]

        gctr = 0
        for j in range(8):
            gs = slice(8 * j, 8 * j + 8)
            self.ld(BSEL, s5b_d[:, :, gs, :], [bBSEL])
            self.ld(CSEL, s5c_d[:, :, gs, :], [bCSEL])
            self.ld(STS5, sts5_v[:, gs, :], [bSTS5])
            rd = [bBSEL, bSC, bBB]
            for (dst, s_a, s_b, ia, ib) in ((0, s_CR, s_S2, 0, 1), (1, s_CI, s_CRN, 0, 1)):
                self.I("dve", "tensor_tensor", rd, [bBB], BB[:, 2], BSEL[:, ia], b3(SC[:, s_a, gs], 16), ALU.mult)
                self.I("dve", "tensor_tensor", rd, [bBB], BB[:, 3], BSEL[:, ib], b3(SC[:, s_b, gs], 16), ALU.mult)
                self.I("dve", "tensor_tensor", rd, [bBB], BB[:, dst], BB[:, 2], BB[:, 3], ALU.add)
            for a in range(2):
                for (dst, g0, g1) in diag_dst(PAD[:, 0]):
                    self.I("dve", "tensor_copy", [bBB], [bPAD], dst, BB[:, a, g0:g1, :])
                for hh in range(2):
                    bk = 3 + hh
                    for q in range(4):
                        gq = hh * 4 + q
                        self.tr(bank[bk][:, q * 128:(q + 1) * 128], PAD[:, 0, gq, :], ident[:], [bPAD, b_ident], [bbank[bk]])
                    self.act(LXY[:, a, hh * 4:(hh + 1) * 4, :], bank[bk].rearrange("p (g m) -> p g m", g=4), AF.Copy,
                             [bbank[bk]], [bLXY])
            for (a, i0, m0, i1, m1) in ((0, 0, MLO, 1, NMHI), (1, 1, NMLO, 0, NMHI)):
                self.I("dve", "tensor_scalar", [bCSEL, b_cvec], [bBB], BB[:, 2], CSEL[:, i0], m0, None, ALU.mult)
                self.I("dve", "scalar_tensor_tensor", [bCSEL, b_cvec, bBB], [bBB], BB[:, 3], CSEL[:, i1], m1, BB[:, 2],
                       ALU.mult, ALU.add)
                for (dst, g0, g1) in diag_dst(L12[:, a]):
                    self.I("dve", "tensor_copy", [bBB], [bL12], dst, BB[:, 3, g0:g1, :])
            self.I("dve", "tensor_copy", [bSC], [bCS], CS8[:, 0, :, 0:1], SC[:, s_COS1, gs].rearrange("p (g o) -> p g o", o=1))
            self.I("dve", "tensor_copy", [bSC], [bCS], CS8[:, 1, :, 0:1], SC[:, s_SIN1, gs].rearrange("p (g o) -> p g o", o=1))
            n = 1
            while n < LCH:
                w = min(n, LCH - n)
                ck = CS8[:, 0, :, n - 1:n].to_broadcast([128, 8, w])
                sk = CS8[:, 1, :, n - 1:n].to_broadcast([128, 8, w])
                co, si = CS8[:, 0, :, 0:w], CS8[:, 1, :, 0:w]
                cn, sn = CS8[:, 0, :, n:n + w], CS8[:, 1, :, n:n + w]
                tt_ = TG[:, 0, :, 0:w]
                rdt = [bCS, bTG]
                self.I("dve", "tensor_tensor", rdt, [bCS], cn, co, ck, ALU.mult)
                self.I("dve", "tensor_tensor", rdt, [bTG], tt_, si, sk, ALU.mult)
                self.I("dve", "tensor_tensor", rdt, [bCS], cn, cn, tt_, ALU.subtract)
                self.I("dve", "tensor_tensor", rdt, [bCS], sn, si, ck, ALU.mult)
                self.I("dve", "tensor_tensor", rdt, [bTG], tt_, co, sk, ALU.mult)
                self.I("dve", "tensor_tensor", rdt, [bCS], sn, sn, tt_, ALU.add)
                n += w
            self.I("dve", "tensor_scalar", [bCS, b_cvec], [bJT], TLS[:, 0, :], CS8[:, 1, :, LCH - 1], NSGN, None, ALU.mult)
            self.I("dve", "tensor_scalar", [bCS, b_cvec], [bJT], TLS[:, 1, :], CS8[:, 1, :, 0], NSGN, None, ALU.mult)
            for a, col in ((0, LCH - 1), (1, 0)):
                for g in range(8):
                    self.I("dve", "tensor_scalar", [bJM, bJT], [bJT], JT, JM, TLS[:, a, g:g + 1], None, ALU.mult)
                    self.I("dve", "scalar_tensor_tensor", [b_ident, bCS, bJT], [bROT], ROT[:, a, g, :], ident[:],
                           CS8[:, 0, g, col:col + 1], JT, ALU.mult, ALU.add)
            if j == 0:
                self.dbg("lxy", LXY.rearrange("p a g m -> p (a g m)"), [bLXY])
                self.dbg("l12", L12.rearrange("p a g m -> p (a g m)"), [bL12])
                self.dbg("cs8", CS8.rearrange("p a g t -> p (a g t)"), [bCS])
                self.dbg("rot", ROT.rearrange("p a g m -> p (a g m)"), [bROT])

            wc, bwc = load_wc(16 + j)

            def cons_p(bi, s, w, ps, bps):
                self.act(USBP[:, s:s + w], ps, AF.Identity, [bps, bBIN], [bUSBP], bias=BIN[:, 16 + j:17 + j], scale=1.0)
            zchunk(wc, bwc, X0Tp, bX0Tp, NPR, cons_p)

            def cons_m(bi, s, w, ps, bps):
                self.act(USF[:, s:s + w], ps, AF.Identity, [bps, bBIN], [bUSF], bias=BIN[:, 16 + j:17 + j], scale=1.0)
            zchunk(wc, bwc, X0T, bX0T, NT, cons_m)
            self.I("pool", "tensor_copy", [bUSF], [bUSB], USB, USF)
            if j == 0:
                self.dbg("usf", USF, [bUSF])

            its = [(ip, k, g) for ip in (0, 1) for k in range(4) for g in range(8)] + [(1, 4, g) for g in range(8)]

            def geom(it):
                ip, k, g = it
                samp = k == 4
                c0, wd = (k * LCH, LCH) if not samp else (NPR, NS)
                return ip, k, g, samp, c0, wd

            def s0(n, it):
                ip, k, g, samp, c0, wd = geom(it)
                xyb = n % 2
                src, bsrc = (USB, bUSB) if ip == 1 else (USBP, bUSBP)
                Xp = psum[:, (2 * xyb) * 512:(2 * xyb) * 512 + wd]
                Yp = psum[:, (2 * xyb + 1) * 512:(2 * xyb + 1) * 512 + wd]
                self.mm(Xp, LXY[:, 0, g, :], src[:, c0:c0 + wd], True, True, [bLXY, bsrc], [bbank[2 * xyb]])
                self.mm(Yp, LXY[:, 1, g, :], src[:, c0:c0 + wd], True, True, [bLXY, bsrc], [bbank[2 * xyb + 1]])

            def s1(n, it):
                ip, k, g, samp, c0, wd = geom(it)
                xyb = n % 2
                XYv = psum[:, (2 * xyb) * 512:(2 * xyb + 2) * 512].rearrange("p (a t) -> p a t", a=2)[:, :, 0:wd]
                cs = CS8[:, :, g, :] if not samp else CS8[:, :, g, 0:1].to_broadcast([128, 2, NS])
                self.I("dve", "tensor_tensor", [bbank[2 * xyb], bbank[2 * xyb + 1], bCS], [bTT[n % 2]], TT[n % 2][:, :, 0:wd], XYv, cs, ALU.mult)

            def s2(n, it):
                ip, k, g, samp, c0, wd = geom(it)
                self.I("pool", "tensor_tensor", [bTT[n % 2]], [bMM[n % 2]], MM_[n % 2][:, 0:wd], TT[n % 2][:, 0, 0:wd], TT[n % 2][:, 1, 0:wd], ALU.add)

            def s3(n, it):
                ip, k, g, samp, c0, wd = geom(it)
                main = ip == 1
                gg = 8 * j + g
                gsb = n % 2
                mm_, bmm = MM_[n % 2], bMM[n % 2]
                if not samp:
                    self.I("dve", "tensor_tensor_scan", [bmm, bSC, bHC[gg]], [bGS[gsb]], GS[gsb],
                           SC[:, s_RHO, gg:gg + 1].to_broadcast([128, LCH]), mm_, HC[:, gg:gg + 1], ALU.mult, ALU.add)
                    self.mm(bank[5][:, 0:1], ROT[:, 0, g, :], GS[gsb][:, LCH - 1:LCH], True, True, [bROT, bGS[gsb]], [bbank[5]])
                    if (not main) and k == 3:
                        self.I("dve", "tensor_scalar", [bbank[5], b_cvec], [bHC[gg]], HC[:, gg:gg + 1], bank[5][:, 0:1], FLAG, None, ALU.mult)
                    elif main and k == 3:
                        self.act(S5OUT[:, gg, 0:1], bank[5][:, 0:1], AF.Copy, [bbank[5]], [bS5OUT])
                    else:
                        self.act(HC[:, gg:gg + 1], bank[5][:, 0:1], AF.Copy, [bbank[5]], [bHC[gg]])
                else:
                    self.I("dve", "scalar_tensor_tensor", [bmm, bSC, bSTS5], [bGS[gsb]], GS[gsb][:, 0:NS], STS5[:, g, :],
                           SC[:, s_RHO, gg:gg + 1], mm_[:, 0:NS], ALU.mult, ALU.add)
                    self.mm(bank[5][:, 0:NS], ROT[:, 1, g, :], GS[gsb][:, 0:NS], True, True, [bROT, bGS[gsb]], [bbank[5]])
                    self.act(S5OUT[:, gg, 1:17], bank[5][:, 0:NS], AF.Copy, [bbank[5]], [bS5OUT])
                if main:
                    cs = CS8[:, :, g, :] if not samp else CS8[:, :, g, 0:1].to_broadcast([128, 2, NS])
                    self.I("pool", "tensor_tensor", [bCS, bGS[gsb]], [bPB[gsb]], PB[gsb][:, :, 0:wd], cs,
                           GS[gsb][:, 0:wd].rearrange("p (o t) -> p o t", o=1).to_broadcast([128, 2, wd]), ALU.mult)

            def s5(n, it):
                ip, k, g, samp, c0, wd = geom(it)
                if ip != 1:
                    return
                gsb = n % 2
                ybk = 6 + (k % 2)
                self.mm(bank[ybk][:, 0:wd], L12[:, 0, g, :], PB[gsb][:, 0, 0:wd], g == 0, False, [bL12, bPB[gsb]], [bbank[ybk]])
                self.mm(bank[ybk][:, 0:wd], L12[:, 1, g, :], PB[gsb][:, 1, 0:wd], False, g == 7, [bL12, bPB[gsb]], [bbank[ybk]])
                if g == 7:
                    self.I("dve", "scalar_tensor_tensor", [bUSF, bDL, bbank[ybk]], [bY5S], Y5S[:, 0:wd], USF[:, c0:c0 + wd],
                           DL[:, j:j + 1], bank[ybk][:, 0:wd], ALU.mult, ALU.add)
                    self.act(G5[:, j, c0:c0 + wd], Y5S[:, 0:wd], AF.Gelu_apprx_tanh, [bY5S], [bG5[j]])
                    self.I("pool", "tensor_copy", [bG5[j]], [bG5b[j]], G5b[:, j, c0:c0 + wd], G5[:, j, c0:c0 + wd])

            NI = len(its)
            s0(0, its[0])
            s0(1, its[1])
            s1(0, its[0])
            s2(0, its[0])
            for n in range(NI):
                if n + 2 < NI:
                    s0(n + 2, its[n + 2])
                if n + 1 < NI:
                    s1(n + 1, its[n + 1])
                    s2(n + 1, its[n + 1])
                s3(n, its[n])
                if n >= 1:
                    s5(n - 1, its[n - 1])
            s5(NI - 1, its[NI - 1])
        self.st(s5out_d, S5OUT.rearrange("p g s -> p (g s)"), [bS5OUT])
        self.dbg("g5", G5.rearrange("p k t -> p (k t)"), bG5)
        if self.stage <= 2:
            return self.finish()
        P.barrier()

        self._ranges = [[R_W + 2104, AW]]
        RGP = self.arena_f(take(64), 64).rearrange("p (h k) -> p h k", h=8)
        bRGP = Buf()
        self.ld(RGP.rearrange("p h k -> p (h k)"), rgp_d, [bRGP])
        GW = self.arena_b(take(1024), 1024).rearrange("p (a h j) -> p a h j", a=2, h=8)
        bGW = Buf()
        self.ld(GW.rearrange("p a h j -> p (a h j)"), rgw_d, [bGW], q="pool")
        STH = self.arena_f(take(128), 128).rearrange("p (h s) -> p h s", h=8)
        STC = self.arena_f(take(384), 384).rearrange("p (h k s) -> p h k s", h=8, k=3)
        bSTH, bSTC = Buf(), Buf()
        self.ld(STH.rearrange("p h s -> p (h s)"), sth_d, [bSTH])
        self.ld(STC.rearrange("p h k s -> p (h k s)"), stc_d, [bSTC])
        RGOUT = self.arena_f(take(136), 136).rearrange("p (h s) -> p h s", h=8)
        CONVOUT = self.arena_f(take(408), 408).rearrange("p (h s) -> p h s", h=8)
        bRGOUT, bCONVOUT = Buf(), Buf()
        C8 = self.arena_f(take(16), 16)
        bC8 = Buf()
        HL = self.arena_f(take(8), 8)
        bHL = Buf()
        XAp = self.arena_f(take(NPR + 3), NPR + 3)
        XA = self.arena_f(take(NT + 3), NT + 3)
        XC = self.arena_f(take(NT), NT)
        XCb = self.arena_b(take(NT // 2), NT // 2)
        Rb_, Ib_, Ab_, Sb_, Hb_, YG = (self.arena_f(take(NT), NT) for _ in range(6))
        bXAp, bXA, bXC, bXCb, bR, bI, bA, bS, bH, bYG = (Buf() for _ in range(10))
        bACTA = [Buf() for _ in range(8)]
        self.act(C8[:, 0:8], RGP[:, :, 7], AF.Exp, [bRGP], [bC8], scale=-1.0)
        self.act(C8[:, 0:8], C8[:, 0:8], AF.Ln, [bC8], [bC8], bias=1.0, scale=1.0)
        self.I("dve", "tensor_scalar", [bC8], [bC8], C8[:, 0:8], C8[:, 0:8], -8.0, None, ALU.mult)
        self.I("dve", "tensor_scalar", [bC8], [bC8], C8[:, 8:16], C8[:, 0:8], 2.0, None, ALU.mult)
        self.I("pool", "memset", [], [bXAp], XAp[:, 0:3], 0.0)

        passes = ((X0Tp, bX0Tp, NPR, XAp, bXAp), (X0T, bX0T, NT, XA, bXA))

        def rg_front(h, ip):
            xT, bxT, n_tok, XAx, bXAx = passes[ip]
            wc, bwc = load_wc(h)

            def cons(bi_, s, w, ps, bps):
                self.act(XAx[:, 3 + s:3 + s + w], ps, AF.Identity, [bps, bBIN], [bXAx], bias=BIN[:, h:h + 1], scale=1.0)
            zchunk(wc, bwc, xT, bxT, n_tok, cons)
            if ip == 1:
                wc2, bwc2 = load_wc(8 + h)

                def cons2(bi_, s, w, ps, bps):
                    self.act(YG[:, s:s + w], ps, AF.Gelu_apprx_tanh, [bps, bBIN], [bYG], bias=BIN[:, 8 + h:9 + h], scale=1.0)
                zchunk(wc2, bwc2, X0T, bX0T, NT, cons2)

        def rg_back(h, ip):
            xT, bxT, n_tok, XAx, bXAx = passes[ip]
            main = ip == 1
            wk = [RGP[:, h, k:k + 1] for k in range(4)]
            cb, ba, bi = RGP[:, h, 4:5], RGP[:, h, 5:6], RGP[:, h, 6:7]
            self.I("dve", "tensor_scalar", [bXAx, bRGP], [bXC], XC[:, 0:NPR], XAx[:, 0:NPR], wk[0], cb, ALU.mult, ALU.add)
            for k in range(1, 4):
                self.I("dve", "scalar_tensor_tensor", [bXAx, bRGP, bXC], [bXC], XC[:, 0:NPR], XAx[:, k:k + NPR], wk[k],
                       XC[:, 0:NPR], ALU.mult, ALU.add)
            if main:
                xs_ = XC[:, NPR:NT]
                self.I("dve", "tensor_scalar", [bSTC, bRGP], [bXC], xs_, STC[:, h, 0, :], wk[0], cb, ALU.mult, ALU.add)
                for k in (1, 2):
                    self.I("dve", "scalar_tensor_tensor", [bSTC, bRGP, bXC], [bXC], xs_, STC[:, h, k, :], wk[k], xs_, ALU.mult, ALU.add)
                self.I("dve", "scalar_tensor_tensor", [bXA, bRGP, bXC], [bXC], xs_, XA[:, 3 + NPR:3 + NT], wk[3], xs_, ALU.mult, ALU.add)
            self.act(XCb[:, :n_tok], XC[:, :n_tok], AF.Copy, [bXC], [bXCb])
            for (which, dstb, bdst, bias_) in ((0, Rb_, bR, ba), (1, Ib_, bI, bi)):
                for bi_, (s, w) in enumerate(_blocks(n_tok)):
                    bk = 3 + (bi_ % 2) + 2 * which
                    self.mm(bank[bk][:, :w], GW[:, which, h, :], XCb[:, s:s + w], True, True, [bGW, bXCb], [bbank[bk]])
                    self.act(dstb[:, s:s + w], bank[bk][:, :w], AF.Sigmoid, [bbank[bk], bRGP], [bdst], bias=bias_, scale=1.0)
            self.act(Ab_[:, :n_tok], Rb_[:, :n_tok], AF.Exp, [bR, bC8], [bA], scale=C8[:, h:h + 1])
            self.act(Sb_[:, :n_tok], Rb_[:, :n_tok], AF.Exp, [bR, bC8], [bS], scale=C8[:, 8 + h:9 + h])
            self.act(Sb_[:, :n_tok], Sb_[:, :n_tok], AF.Sqrt, [bS], [bS], bias=1.0, scale=-1.0)
            self.I("dve", "tensor_tensor", [bI, bXC], [bI], Ib_[:, :n_tok], Ib_[:, :n_tok], XC[:, :n_tok], ALU.mult)
            self.I("dve", "tensor_tensor", [bI, bS], [bI], Ib_[:, :n_tok], Ib_[:, :n_tok], Sb_[:, :n_tok], ALU.mult)
            if not main:
                self.I("dve", "tensor_tensor_scan", [bA, bI], [bH], Hb_[:, 0:NPR], Ab_[:, 0:NPR], Ib_[:, 0:NPR], 0.0, ALU.mult, ALU.add)
                self.I("dve", "tensor_scalar", [bH, b_cvec], [bHL], HL[:, h:h + 1], Hb_[:, NPR - 1:NPR], FLAG, None, ALU.mult)
                self.I("dve", "tensor_scalar", [bXAp, b_cvec], [bXA], XA[:, 0:3], XAp[:, NPR:NPR + 3], FLAG, None, ALU.mult)
            else:
                self.I("dve", "tensor_tensor_scan", [bA, bI, bHL], [bH], Hb_[:, 0:NPR], Ab_[:, 0:NPR], Ib_[:, 0:NPR], HL[:, h:h + 1],
                       ALU.mult, ALU.add)
                self.I("dve", "tensor_tensor", [bSTH, bA], [bH], Hb_[:, NPR:NT], STH[:, h, :], Ab_[:, NPR:NT], ALU.mult)
                self.I("dve", "tensor_tensor", [bH, bI], [bH], Hb_[:, NPR:NT], Hb_[:, NPR:NT], Ib_[:, NPR:NT], ALU.add)
                self.act(RGOUT[:, h, :], Hb_[:, NPR - 1:NT], AF.Copy, [bH], [bRGOUT])
                self.act(CONVOUT[:, h, 0:3], XA[:, NPR:NPR + 3], AF.Copy, [bXA], [bCONVOUT])
                cv = CONVOUT[:, h, 3:51].rearrange("p (s k) -> p s k", k=3)
                self.act(cv[:, :, 0], STC[:, h, 1, :], AF.Copy, [bSTC], [bCONVOUT])
                self.act(cv[:, :, 1], STC[:, h, 2, :], AF.Copy, [bSTC], [bCONVOUT])
                self.act(cv[:, :, 2], XA[:, 3 + NPR:3 + NT], AF.Copy, [bXA], [bCONVOUT])
                self.I("dve", "tensor_tensor", [bH, bYG], [bACTA[h]], ACTA[:, h, :], Hb_, YG, ALU.mult)

        rg_its = [(h, ip) for h in range(8) for ip in (0, 1)]
        rg_front(*rg_its[0])
        for i, it in enumerate(rg_its):
            if i + 1 < len(rg_its):
                rg_front(*rg_its[i + 1])
            rg_back(*it)
        self.st(rgout_d, RGOUT.rearrange("p h s -> p (h s)"), [bRGOUT])
        self.st(convout_d, CONVOUT.rearrange("p h s -> p (h s)"), [bCONVOUT])
        self.dbg("acta", ACTA.rearrange("p k t -> p (k t)"), bACTA)
        if self.stage <= 3:
            return self.finish()
        P.barrier()

        self._ranges = [[R_W + 2104, AW]]
        GLUb = self.arena_b(R_B, 4192).rearrange("p (k t) -> p k t", k=8)
        bGLUb = [Buf() for _ in range(8)]
        GWT = [self.arena_b(take(512), 512).rearrange("p (k m) -> p k m", k=8) for _ in range(2)]
        bGWT = [Buf(), Buf()]
        SG = [self.arena_f(take(512), 512) for _ in range(2)]
        bSG = [Buf(), Buf()]
        GLB = self.arena_f(take(8), 8)
        bGLB = Buf()
        self.ld(GLB, glub_d, [bGLB])
        ctr = 0
        for n in range(8):
            gw, bgw = GWT[n % 2], bGWT[n % 2]
            self.ld(gw.rearrange("p k m -> p (k m)"), gluw_d[n], [bgw], q="pool")
            for bi_, (s, w) in enumerate(_blocks(NT)):
                bk = ctr % 4
                sg, bsg = SG[ctr % 2], bSG[ctr % 2]
                ctr += 1
                for kc in range(8):
                    self.mm(bank[bk][:, :w], gw[:, kc, :], G5b[:, kc, s:s + w], kc == 0, kc == 7, [bgw, bG5b[kc]], [bbank[bk]])
                self.act(sg[:, :w], bank[bk][:, :w], AF.Sigmoid, [bbank[bk], bGLB], [bsg], bias=GLB[:, n:n + 1], scale=1.0)
                self.I("dve", "tensor_tensor", [bG5[n], bsg], [bGLUb[n]], GLUb[:, n, s:s + w], G5[:, n, s:s + w], sg[:, :w], ALU.mult)
        self.dbg("glub", GLUb.rearrange("p k t -> p (k t)"), bGLUb)
        if self.stage <= 4:
            return self.finish()
        P.barrier()

        self._ranges = [[R_W + 2104, AW]]
        MT = self.arena_b(R_C, 8384).rearrange("p (k t) -> p k t", k=16)
        bMT = [Buf() for _ in range(16)]
        PAW = [self.arena_b(take(512), 512).rearrange("p (k m) -> p k m", k=8) for _ in range(2)]
        PBW = [self.arena_b(take(512), 512).rearrange("p (k m) -> p k m", k=8) for _ in range(2)]
        bPAW, bPBW = [Buf(), Buf()], [Buf(), Buf()]
        WG = [self.arena_b(take(1024), 1024).rearrange("p (k m) -> p k m", k=16) for _ in range(4)]
        bWG = [Buf() for _ in range(4)]
        T1 = [self.arena_f(take(512), 512) for _ in range(2)]
        T2 = [self.arena_f(take(512), 512) for _ in range(2)]
        bT1, bT2 = [Buf(), Buf()], [Buf(), Buf()]
        ctr = 0
        for m in range(16):
            pa, bpa, pb, bpb = PAW[m % 2], bPAW[m % 2], PBW[m % 2], bPBW[m % 2]
            wga, bwga, wgb, bwgb = WG[2 * (m % 2)], bWG[2 * (m % 2)], WG[2 * (m % 2) + 1], bWG[2 * (m % 2) + 1]
            self.ld(pa.rearrange("p k m -> p (k m)"), pa_d[m], [bpa], q="pool")
            self.ld(pb.rearrange("p k m -> p (k m)"), pb_d[m], [bpb], q="pool")
            self.ld(wga.rearrange("p k m -> p (k m)"), w_in_l[24 + m], [bwga], q="pool")
            self.ld(wgb.rearrange("p k m -> p (k m)"), w_in_l[40 + m], [bwgb], q="pool")
            for bi_, (s, w) in enumerate(_blocks(NT)):
                par = ctr % 2
                ctr += 1
                kA, kB, kGA, kGB = 4 * par, 4 * par + 1, 4 * par + 2, 4 * par + 3
                tl = sorted(set(range(s // 128, (s + w - 1) // 128 + 1)))
                rx = [bX0T[t] for t in tl]
                for kc in range(8):
                    self.mm(bank[kA][:, :w], pa[:, kc, :], ACTA[:, kc, s:s + w], kc == 0, kc == 7, [bpa, bACTA[kc]], [bbank[kA]])
                for kc in range(8):
                    self.mm(bank[kB][:, :w], pb[:, kc, :], GLUb[:, kc, s:s + w], kc == 0, kc == 7, [bpb, bGLUb[kc]], [bbank[kB]])
                for kc in range(16):
                    self.mm(bank[kGA][:, :w], wga[:, kc, :], X0T[:, kc, s:s + w], kc == 0, kc == 15, [bwga] + rx, [bbank[kGA]])
                for kc in range(16):
                    self.mm(bank[kGB][:, :w], wgb[:, kc, :], X0T[:, kc, s:s + w], kc == 0, kc == 15, [bwgb] + rx, [bbank[kGB]])
                t1, bt1, t2, bt2 = T1[par], bT1[par], T2[par], bT2[par]
                self.act(t1[:, :w], bank[kGA][:, :w], AF.Sigmoid, [bbank[kGA], bBIN], [bt1], bias=BIN[:, 24 + m:25 + m], scale=1.0)
                self.act(t2[:, :w], bank[kGB][:, :w], AF.Sigmoid, [bbank[kGB], bBIN], [bt2], bias=BIN[:, 40 + m:41 + m], scale=1.0)
                self.I("dve", "tensor_tensor", [bt1, bbank[kA]], [bt1], t1[:, :w], t1[:, :w], bank[kA][:, :w], ALU.mult)
                self.I("dve", "tensor_tensor", [bt2, bbank[kB]], [bt2], t2[:, :w], t2[:, :w], bank[kB][:, :w], ALU.mult)
                self.I("pool", "tensor_tensor", [bt1, bt2], [bMT[m]], MT[:, m, s:s + w], t1[:, :w], t2[:, :w], ALU.add)
        self.dbg("mt", MT.rearrange("p k t -> p (k t)"), bMT)
        if self.stage <= 5:
            return self.finish()
        P.barrier()

        TILES = _tiles(NT)
        X1 = [self.arena_f(R_D + i * 2048, 2048) for i in range(9)]
        bX1 = [Buf(f"x1_{i}") for i in range(9)]
        X1T = self.arena_b(R_A, 8384).rearrange("p (k t) -> p k t", k=16)
        bX1T = [Buf(f"x1t{i}") for i in range(9)]
        WO = self.arena_b(R_B, 4096).rearrange("p (k n) -> p k n", k=16)
        bWO = Buf()
        LNG1 = self.arena_f(R_B + 4096, 2048)
        LNB1 = self.arena_f(R_B + 6144, 2048)
        bLN1 = Buf()
        so = R_D + 9 * 2048
        XT1 = self.arena_f(so, 2048)
        bXT1 = Buf()
        LNG0 = self.arena_f(so + 2048, 2048)
        LNB0 = self.arena_f(so + 4096, 2048)
        bLN0 = Buf()
        XB = self.arena_b(so + 6144, 1024)
        bXB = Buf()
        STAT = self.arena_f(so + 7168, 32)
        bSTAT = Buf()
        assert so + 7200 <= AW
        self.ld(LNG0, lnp[0:1, :].partition_broadcast(128), [bLN0])
        self.ld(LNB0, lnp[1:2, :].partition_broadcast(128), [bLN0])
        self.ld(LNG1, lnp[2:3, :].partition_broadcast(128), [bLN1])
        self.ld(LNB1, lnp[3:4, :].partition_broadcast(128), [bLN1])
        for f in range(4):
            self.ld(WO.rearrange("p k n -> p (k n)").rearrange("p (a b) -> p a b", a=4),
                    wo_d[f].rearrange("p (a b) -> p a b", a=4), [bWO], q="pool")
            for i, (r0, rows) in enumerate(TILES):
                bk = i % 4
                for m in range(16):
                    self.mm(bank[bk][:rows, :], MT[:, m, r0:r0 + rows], WO[:, m, :], m == 0, m == 15, [bMT[m], bWO], [bbank[bk]])
                self.act(X1[i][:rows, f * 512:(f + 1) * 512], bank[bk][:rows, :], AF.Copy, [bbank[bk]], [bX1[i]])

        def ln_inplace(x, bx, rows, lng, lnb, bln, STAT=STAT, bSTAT=bSTAT):
            st6 = STAT[:, 0:24].rearrange("p (c s) -> p c s", c=4)
            for c in range(4):
                self.I("dve", "bn_stats", [bx], [bSTAT], st6[:rows, c, :], x[:rows, c * 512:(c + 1) * 512])
            self.I("dve", "bn_aggr", [bSTAT], [bSTAT], STAT[:rows, 24:26], STAT[:rows, 0:24])
            self.act(STAT[:rows, 26:27], STAT[:rows, 25:26], AF.Sqrt, [bSTAT], [bSTAT], bias=EPS, scale=1.0)
            self.I("dve", "reciprocal", [bSTAT], [bSTAT], STAT[:rows, 27:28], STAT[:rows, 26:27])
            self.I("dve", "tensor_scalar", [bx, bSTAT], [bx], x[:rows], x[:rows], STAT[:rows, 24:25],
                   STAT[:rows, 27:28], ALU.subtract, ALU.mult)
            self.I("pool", "tensor_tensor", [bx, bln], [bx], x[:rows], x[:rows], lng[:rows], ALU.mult)
            self.I("dve", "tensor_tensor", [bx, bln], [bx], x[:rows], x[:rows], lnb[:rows], ALU.add)
        self.ln_inplace = ln_inplace

        XT1s = [XT1, self.arena_f(R_C, 2048)]
        bXT1s = [bXT1, Buf()]
        XBs = [XB, self.arena_b(R_C + 2048, 1024)]
        bXBs = [bXB, Buf()]
        for i, (r0, rows) in enumerate(TILES):
            xt1, bxt1 = XT1s[i % 2], bXT1s[i % 2]
            extra = list(bMT) if i == 1 else []
            self.P.dma("sp", lambda e, xt1=xt1, r0=r0, rows=rows: e.dma_start(out=xt1[:rows], in_=xmain[r0:r0 + rows, :]),
                       [], [bxt1] + extra, chan=bxt1)
            ln_inplace(xt1, bxt1, rows, LNG0, LNB0, bLN0)
            self.I("dve", "scalar_tensor_tensor", [bxt1, bX1[i]], [bX1[i]], X1[i][:rows], xt1[:rows], ALPHA, X1[i][:rows], ALU.mult, ALU.add)
            ln_inplace(X1[i], bX1[i], rows, LNG1, LNB1, bLN1)
            xb_, bxb_ = XBs[i % 2], bXBs[i % 2]
            wr = [bxb_] + (list(bMT) if i == 1 else [])
            self.act(xb_[:rows], X1[i][:rows], AF.Copy, [bX1[i]], wr)
            transpose_tile(xb_, bxb_, rows, X1T, r0, bX1T[i])
        for i, (r0, rows) in enumerate(TILES):
            self.dbg(f"x1_{i}", X1[i][:rows], [bX1[i]])
        if self.stage <= 6:
            return self.finish()
        P.barrier()

        AX = mybir.AxisListType
        so = R_D + 9 * 2048
        self._ranges = [[so, AW]]
        SL = [self.arena_b(R_B + q * 4096, 4096) for q in range(4)]
        bSL = [Buf(f"slot{q}") for q in range(4)]
        Hh = [self.arena_b(take(2096), 2096).rearrange("p (k t) -> p k t", k=4) for _ in range(2)]
        bHh = [[Buf() for _ in range(3)] for _ in range(2)]
        Ss = [self.arena_f(take(512), 512) for _ in range(2)]
        bSs = [Buf(), Buf()]
        GATES = [self.arena_f(take(NE + 1), NE + 1) for _ in range(9)]
        bGATES = [Buf(f"gates{i}") for i in range(9)]
        RW = self.arena_b(take(512), 512).rearrange("p (k n) -> p k n", k=16)
        bRW = Buf()
        RB = self.arena_f(take(64), 64)
        bRB = Buf()
        STAT2 = self.arena_f(take(32), 32)
        RS = self.arena_f(take(64 * 5 + 8 * 5 + 8), 64 * 5 + 48)
        bRS = Buf("rs")
        SCO, SELB, GTOP, MASKED, EM = (RS[:, i * 64:(i + 1) * 64] for i in range(5))
        GSC, GS8, GMASK, PEN, TOP8, WS = (RS[:, 320 + i * 8:328 + i * 8] for i in range(6))
        self.ld(RW.rearrange("p k n -> p (k n)"), rw_d.rearrange("(p kc) n -> p (kc n)", kc=16), [bRW], q="pool")
        self.ld(RB, rb_d.partition_broadcast(128), [bRB])
        for i in range(9):
            self.I("pool", "memset", [], [bGATES[i]], GATES[i][:, NE:NE + 1], 1.0)

        def wsrc(e, which):
            if e < NE:
                d = (w1_d, w3_d, w2_d)[which][e]
            else:
                d = (sw1_d, sw3_d, sw2_d)[which]
            if which < 2:
                return d.rearrange("(p kc) n -> p (kc n)", kc=16).rearrange("p (a b) -> p a b", a=4)
            return d.rearrange("(jc p) n -> p jc n", p=128)

        NEX = NE + 1

        def load_w(idx):
            e, which = divmod(idx, 3)
            if e >= NEX:
                return
            q = idx % 4
            self.ld(SL[q].rearrange("p (a b) -> p a b", a=4), wsrc(e, which), [bSL[q]], q="pool")

        for idx in range(4):
            load_w(idx)

        SELB3 = SELB.rearrange("p (g k) -> p g k", g=8)
        GTOP3 = GTOP.rearrange("p (g k) -> p g k", g=8)
        MASKED3 = MASKED.rearrange("p (g k) -> p g k", g=8)
        rr = [bRS]
        def router_tile(i):
            r0, rows = TILES[i]
            bk = 4 + (i % 2)
            for kc in range(16):
                self.mm(bank[bk][:rows, 0:64], X1T[:, kc, r0:r0 + rows], RW[:, kc, :], kc == 0, kc == 15, [bX1T[i], bRW], [bbank[bk]])
            self.I("pool", "tensor_scalar", [bX1[i]], [bX1[i]], X1[i][:rows], X1[i][:rows], ALPHA, None, ALU.mult)
            self.act(SCO[:rows], bank[bk][:rows, 0:64], AF.Sigmoid, [bbank[bk]] + rr, rr)
            self.I("dve", "tensor_tensor", rr + [bRB], rr, SELB[:rows], SCO[:rows], RB[:rows], ALU.add)
            for g in range(8):
                self.I("dve", "max", rr, rr, GTOP[:rows, g * 8:(g + 1) * 8], SELB[:rows, g * 8:(g + 1) * 8])
            self.I("dve", "tensor_tensor", rr, rr, GSC[:rows], GTOP3[:rows, :, 0], GTOP3[:rows, :, 1], ALU.add)
            self.I("dve", "max", rr, rr, GS8[:rows], GSC[:rows])
            self.I("dve", "tensor_scalar", rr, rr, GMASK[:rows], GSC[:rows], GS8[:rows, 3:4], None, ALU.is_ge)
            self.I("dve", "tensor_scalar", rr, rr, PEN[:rows], GMASK[:rows], -1.0, 1e30, ALU.add, ALU.mult)
            self.I("dve", "tensor_tensor", rr, rr, MASKED3[:rows], SELB3[:rows], b3(GMASK[:rows], 8), ALU.mult)
            self.I("dve", "tensor_tensor", rr, rr, MASKED3[:rows], MASKED3[:rows], b3(PEN[:rows], 8), ALU.add)
            self.I("dve", "max", rr, rr, TOP8[:rows], MASKED[:rows])
            self.I("dve", "tensor_scalar", rr, rr, EM[:rows], MASKED[:rows], TOP8[:rows, 7:8], None, ALU.is_ge)
            self.I("dve", "tensor_tensor", rr, rr, EM[:rows], EM[:rows], SCO[:rows], ALU.mult)
            self.I("dve", "reduce_sum", rr, rr, WS[:rows, 0:1], EM[:rows], AX.X)
            self.I("dve", "reciprocal", rr, rr, WS[:rows, 1:2], WS[:rows, 0:1])
            self.I("dve", "tensor_scalar", rr, [bGATES[i]], GATES[i][:rows, 0:NE], EM[:rows], WS[:rows, 1:2], 2.5, ALU.mult, ALU.mult)

        self._rt_next = 0
        BLK = [(0, 384), (384, 384), (768, 280)]
        blk_tiles = {0: [0, 1, 2], 1: [3, 4, 5], 2: [6, 7, 8]}
        octr = 0
        for e in range(NEX):
            par = e % 2
            q1, q3, q2 = (3 * e) % 4, (3 * e + 1) % 4, (3 * e + 2) % 4
            W1 = SL[q1].rearrange("p (k n) -> p k n", k=16)
            W3 = SL[q3].rearrange("p (k n) -> p k n", k=16)
            W2 = SL[q2].rearrange("p (k n) -> p k n", k=4)

            def ab(bi):
                s, w = BLK[bi]
                tl = blk_tiles[bi]
                rx = [bX1T[t] for t in tl]
                for jc in range(4):
                    ka, kb = jc % 2, 2 + (jc % 2)
                    for kc in range(16):
                        self.mm(bank[ka][:, :w], W1[:, kc, jc * 128:(jc + 1) * 128], X1T[:, kc, s:s + w], kc == 0, kc == 15,
                                [bSL[q1]] + rx, [bbank[ka]])
                    for kc in range(16):
                        self.mm(bank[kb][:, :w], W3[:, kc, jc * 128:(jc + 1) * 128], X1T[:, kc, s:s + w], kc == 0, kc == 15,
                                [bSL[q3]] + rx, [bbank[kb]])
                    ss_, bss = Ss[jc % 2], bSs[jc % 2]
                    self.act(ss_[:, :w], bank[ka][:, :w], AF.Silu, [bbank[ka]], [bss])
                    self.I("dve", "tensor_tensor", [bss, bbank[kb]], [bHh[par][bi]], Hh[par][:, jc, s:s + w], ss_[:, :w],
                           bank[kb][:, :w], ALU.mult)
                    if e == 0 and self._rt_next < 9:
                        router_tile(self._rt_next)
                        self._rt_next += 1

            def w2p(bi):
                nonlocal octr
                for t in blk_tiles[bi]:
                    r0, rows = TILES[t]
                    for f in range(4):
                        ok = 4 + (octr % 4)
                        octr += 1
                        for jc in range(4):
                            self.mm(bank[ok][:rows, :], Hh[par][:, jc, r0:r0 + rows], W2[:, jc, f * 512:(f + 1) * 512], jc == 0, jc == 3,
                                    [bHh[par][bi], bSL[q2]], [bbank[ok]])
                        self.I("dve", "scalar_tensor_tensor", [bX1[t], bbank[ok], bGATES[t]], [bX1[t]], X1[t][:rows, f * 512:(f + 1) * 512],
                               bank[ok][:rows, :], GATES[t][:rows, e:e + 1], X1[t][:rows, f * 512:(f + 1) * 512], ALU.mult, ALU.add)

            last = e == NEX - 1
            if last:
                assert 3 not in (q1, q3, q2)
                LNG2 = self.arena_f(R_B + 3 * 4096, 2048)
                LNB2 = self.arena_f(R_B + 3 * 4096 + 2048, 2048)
                self.ld(LNG2, lnp[4:5, :].partition_broadcast(128), [bSL[3]])
                self.ld(LNB2, lnp[5:6, :].partition_broadcast(128), [bSL[3]])
                bST2 = Buf()

            def ln2_tiles(bi):
                for t in blk_tiles[bi]:
                    r0, rows = TILES[t]
                    ln_inplace(X1[t], bX1[t], rows, LNG2, LNB2, bSL[3], STAT2, bST2)
                    self.st(y_d[r0:r0 + rows, :], X1[t][:rows], [bX1[t]])

            ab(0)
            ab(1)
            w2p(0)
            ab(2)
            load_w(3 * e + 4)
            load_w(3 * e + 5)
            if last:
                ln2_tiles(0)
            w2p(1)
            if last:
                ln2_tiles(1)
            w2p(2)
            if last:
                ln2_tiles(2)
            load_w(3 * e + 6)
        return self.finish()

    def finish(self):
        self.P.emit(self.final)
        return self.nc


def _c(a):
    return np.ascontiguousarray(a, dtype=np.float32)


def prep_inputs(inp):
    g = {k: np.asarray(v) for k, v in inp.items()}
    xp = g["x_prompt"]
    meta = g["meta_tokens"]
    xs = g["x_sample"][:, 0, :]
    p = np.arange(128)
    cvec = np.zeros((128, 8), np.float32)
    cvec[:, 1] = np.where(p < 64, -1.0, 1.0)
    cvec[:, 2] = -cvec[:, 1]
    cvec[:, 3] = (p < 64)
    cvec[:, 4] = -(p >= 64).astype(np.float32)
    cvec[:, 5] = -(p < 64).astype(np.float32)
    shared = {}
    shared["ident"] = np.eye(128, dtype=np.float32)
    shared["lnp"] = _c(np.stack([g["ln_in_g"], g["ln_in_b"], g["ln1_g"][0], g["ln1_b"][0], g["ln2_g"][0], g["ln2_b"][0]]))
    w_in = g["w_in"][0]
    shared["w_in_l"] = _c(w_in.reshape(128, 16, 56, 128).transpose(2, 0, 1, 3).reshape(56, 128, 2048))
    shared["b_in_l"] = _c(g["b_in"][0].reshape(56, 128).T)
    rgp = np.zeros((128, 8, 8), np.float32)
    cw = g["conv_w"][0].reshape(4, 8, 128)
    for k in range(4):
        rgp[:, :, k] = cw[k].T
    rgp[:, :, 4] = g["conv_b"][0].reshape(8, 128).T
    rgp[:, :, 5] = g["rg_ba"][0].reshape(8, 128).T
    rgp[:, :, 6] = g["rg_bi"][0].reshape(8, 128).T
    rgp[:, :, 7] = g["rg_lambda"][0].reshape(8, 128).T
    shared["rgp"] = _c(rgp.reshape(128, 64))
    rgw = np.stack([g["rg_wa"][0].transpose(1, 0, 2), g["rg_wi"][0].transpose(1, 0, 2)], axis=1)
    shared["rg_w"] = _c(rgw.reshape(128, 2 * 8 * 128))
    two = lambda a: np.concatenate([a, a], axis=0)
    shared["s5sc"] = _c(np.stack([two(g["s5_a_re"][0].T), two(g["s5_a_im"][0].T),
                                  np.broadcast_to(g["s5_log_dt"][0][None, :], (128, 64))], axis=1).reshape(128, 192))
    bre = g["s5_b_re"][0].transpose(1, 0, 2)
    bim = g["s5_b_im"][0].transpose(1, 0, 2)
    shared["s5b"] = _c(np.stack([np.concatenate([bre, bim], 0), np.concatenate([bim, bre], 0)], axis=1))
    cre = g["s5_c_re"][0].transpose(2, 0, 1)
    cim = g["s5_c_im"][0].transpose(2, 0, 1)
    shared["s5c"] = _c(np.stack([two(cre), two(cim)], axis=1))
    shared["s5d"] = _c(g["s5_d"][0].reshape(8, 128).T)
    shared["glu_w_l"] = _c(g["glu_w"][0].reshape(8, 128, 8, 128).transpose(2, 1, 0, 3).reshape(8, 128, 1024))
    shared["glu_b_l"] = _c(g["glu_b"][0].reshape(8, 128).T)
    shared["proj_a_l"] = _c(g["proj_a"][0].reshape(8, 128, 16, 128).transpose(2, 1, 0, 3).reshape(16, 128, 1024))
    shared["proj_b_l"] = _c(g["proj_b"][0].reshape(8, 128, 16, 128).transpose(2, 1, 0, 3).reshape(16, 128, 1024))
    shared["w_o_l"] = _c(g["w_o"][0].reshape(16, 128, 4, 512).transpose(2, 1, 0, 3).reshape(4, 128, 16 * 512))
    shared["router_w"] = _c(g["router_w"][0])
    shared["router_bias"] = _c(g["router_bias"][0][None, :])
    shared["ex_w1"] = _c(g["ex_w1"][0])
    shared["ex_w3"] = _c(g["ex_w3"][0])
    shared["ex_w2"] = _c(g["ex_w2"][0])
    shared["sh_w1"] = _c(g["sh_w1"][0])
    shared["sh_w3"] = _c(g["sh_w3"][0])
    shared["sh_w2"] = _c(g["sh_w2"][0])
    maps = []
    for c in range(8):
        b, half = c // 2, c % 2
        full = np.concatenate([meta, xp[b]], axis=0)
        m = dict(shared)
        own = full[half * NPR:(half + 1) * NPR]
        ss = slice(c * NS, (c + 1) * NS)
        m["xmain"] = _c(np.concatenate([own, xs[ss]], axis=0))
        m["xprev"] = _c(full[0:NPR]) if half == 1 else np.zeros((NPR, D), np.float32)
        cv = cvec.copy()
        cv[:, 0] = float(half)
        m["cvec"] = cv
        sh = g["state_rglru_h"][0][ss]
        m["st_h"] = _c(sh.reshape(NS, 8, 128).transpose(2, 1, 0).reshape(128, 128))
        sc = g["state_conv"][0][ss]
        m["st_conv"] = _c(sc.reshape(NS, 3, 8, 128).transpose(3, 2, 1, 0).reshape(128, 8 * 3 * 16))
        sr = g["state_s5_re"][0][ss].transpose(2, 1, 0)
        si = g["state_s5_im"][0][ss].transpose(2, 1, 0)
        m["st_s5"] = _c(np.concatenate([sr, si], axis=0).reshape(128, 64 * 16))
        maps.append(m)
    return maps


_NC_CACHE = {}


def _get_nc():
    if "nc" not in _NC_CACHE:
        k = K()
        _NC_CACHE["nc"] = k.build()
    return _NC_CACHE["nc"]


def kernel(**inputs):
    maps = prep_inputs(inputs)
    nc = _get_nc()
    res = run_bass_kernel_spmd(nc, maps, core_ids=list(range(8)))
    R = res.results
    f32 = np.float32
    y_prompt = np.zeros((4, 2048, D), f32)
    y_sample = np.zeros((128, 1, D), f32)
    p_h = np.zeros((1, 4, DR), f32)
    p_c = np.zeros((1, 4, 3, DR), f32)
    p_r = np.zeros((1, 4, 64, 64), f32)
    p_i = np.zeros((1, 4, 64, 64), f32)
    s_h = np.zeros((1, 128, DR), f32)
    s_c = np.zeros((1, 128, 3, DR), f32)
    s_r = np.zeros((1, 128, 64, 64), f32)
    s_i = np.zeros((1, 128, 64, 64), f32)
    for c in range(8):
        b, half = c // 2, c % 2
        y = np.asarray(R[c]["y"])
        if half == 0:
            y_prompt[b, 0:NPR - 16] = y[16:NPR]
        else:
            y_prompt[b, NPR - 16:2048] = y[0:NPR]
        ss = slice(c * NS, (c + 1) * NS)
        y_sample[ss, 0] = y[NPR:NT]
        rg = np.asarray(R[c]["rgout"]).reshape(128, 8, 17)
        cv = np.asarray(R[c]["convout"]).reshape(128, 8, 51)
        s5 = np.asarray(R[c]["s5out"]).reshape(128, 64, 17)
        s_h[0, ss] = rg[:, :, 1:].transpose(2, 1, 0).reshape(NS, DR)
        s_c[0, ss] = cv[:, :, 3:].reshape(128, 8, NS, 3).transpose(2, 3, 1, 0).reshape(NS, 3, DR)
        s_r[0, ss] = s5[:64, :, 1:].transpose(2, 1, 0)
        s_i[0, ss] = s5[64:, :, 1:].transpose(2, 1, 0)
        if half == 1:
            p_h[0, b] = rg[:, :, 0].T.reshape(DR)
            p_c[0, b] = cv[:, :, 0:3].transpose(2, 1, 0).reshape(3, DR)
            p_r[0, b] = s5[:64, :, 0].T
            p_i[0, b] = s5[64:, :, 0].T
    return (y_prompt, y_sample, p_h, p_c, p_r, p_i, s_h, s_c, s_r, s_i)
```
